# Optimizing a Trainium2 kernel written in Bass

```python
import math
import jax, jax.numpy as jnp
from jax import lax
import numpy as np

D_MODEL = 1024
BATCH = 16
SEQ = 2048
DEPTH = 1

PLE_DIM = 256
RWKV_HEADS = 8
RWKV_HEAD_DIM = 64
RWKV_WIDTH = RWKV_HEADS * RWKV_HEAD_DIM
DECAY_LORA = 64
AAA_LORA = 64
GATE_LORA = 128
RWKV_COLS = 3 * RWKV_WIDTH + DECAY_LORA + AAA_LORA + GATE_LORA
S5_GROUPS = 16
S5_GROUP_CH = 16
S5_WIDTH = S5_GROUPS * S5_GROUP_CH
S5_STATE = 64
IN_COLS = RWKV_COLS + S5_WIDTH + 2 * D_MODEL
N_GROUPS = 4
EXPERTS_PER_GROUP = 8
N_EXPERTS = N_GROUPS * EXPERTS_PER_GROUP
TOP_K = 2
D_EXPERT = 256

NORM_EPS = 1e-6
GN_EPS = 64e-5

kernel_name = "hybrid_rwkv7_s5_hmoe_block"


def rms_norm(x, g):
    xf = x.astype(jnp.float32)
    y = xf * lax.rsqrt(jnp.mean(xf * xf, axis=-1, keepdims=True) + NORM_EPS)
    return (y * g.astype(jnp.float32)).astype(x.dtype)


def wkv7_scan(r, decay, k, v, a, b):
    bsz, _, h, n = r.shape

    def step(state, xs):
        r_t, w_t, k_t, v_t, a_t, b_t = xs
        sa = jnp.einsum('bhvk,bhk->bhv', state, a_t)
        state = (state * w_t[:, :, None, :]
                 + sa[..., :, None] * b_t[:, :, None, :]
                 + v_t[..., :, None] * k_t[:, :, None, :])
        return state, jnp.einsum('bhvk,bhk->bhv', state, r_t)

    xs = tuple(jnp.swapaxes(t, 0, 1) for t in (r, decay, k, v, a, b))
    state0 = jnp.zeros((bsz, h, n, n), jnp.float32)
    _, y = lax.scan(step, state0, xs)
    return jnp.swapaxes(y, 0, 1)


def rwkv7_branch(cols, w0, w_up, a0, a_up, g_up, k_k, k_a, r_k, ln_g, ln_b):
    bsz, s, _ = cols.shape
    W = RWKV_WIDTH
    r, k, v, wd, ad, gd = jnp.split(
        cols, [W, 2 * W, 3 * W, 3 * W + DECAY_LORA, 3 * W + DECAY_LORA + AAA_LORA], axis=-1)
    w_log = -jax.nn.softplus(-(w0 + jnp.tanh(wd) @ w_up)) - 0.5
    decay = jnp.exp(-jnp.exp(w_log.astype(jnp.float32)))
    a = jax.nn.sigmoid(a0 + ad @ a_up)
    g = jax.nn.sigmoid(gd) @ g_up
    heads = lambda t: t.astype(jnp.float32).reshape(bsz, s, RWKV_HEADS, RWKV_HEAD_DIM)
    kk = heads(k * k_k)
    kk = kk / jnp.maximum(jnp.sqrt(jnp.sum(kk * kk, axis=-1, keepdims=True)), 1e-12)
    k = k * (1.0 + (a - 1.0) * k_a)
    r_h, k_h, v_h, a_h, w_h = heads(r), heads(k), heads(v), heads(a), heads(decay)
    y = wkv7_scan(r_h, w_h, k_h, v_h, -kk, kk * a_h)
    mu = jnp.mean(y, axis=-1, keepdims=True)
    var = jnp.mean(jnp.square(y - mu), axis=-1, keepdims=True)
    y = ((y - mu) * lax.rsqrt(var + GN_EPS)).reshape(bsz, s, W)
    y = y * ln_g.astype(jnp.float32) + ln_b.astype(jnp.float32)
    bonus = jnp.sum(r_h * k_h * r_k.astype(jnp.float32), axis=-1, keepdims=True) * v_h
    y = y + bonus.reshape(bsz, s, W)
    return (y * g.astype(jnp.float32)).astype(cols.dtype)


def complex_linear_combine(e1, e2):
    a1r, a1i, b1r, b1i = e1
    a2r, a2i, b2r, b2i = e2
    ar = a2r * a1r - a2i * a1i
    ai = a2r * a1i + a2i * a1r
    br = a2r * b1r - a2i * b1i + b2r
    bi = a2r * b1i + a2i * b1r + b2i
    return ar, ai, br, bi


def s5_branch(u, lam_re, lam_im, log_dt, b_re, b_im, c_re, c_im, d_skip, glu_w, glu_b):
    bsz, s, _ = u.shape
    uf = u.astype(jnp.float32).reshape(bsz, s, S5_GROUPS, S5_GROUP_CH)
    dt = jnp.exp(log_dt.astype(jnp.float32))[:, None]
    lr, li = lam_re.astype(jnp.float32), lam_im.astype(jnp.float32)
    mag = jnp.exp(lr * dt)
    lb_re, lb_im = mag * jnp.cos(li * dt), mag * jnp.sin(li * dt)
    den = lr * lr + li * li
    nr, ni = lb_re - 1.0, lb_im
    coef_re = (nr * lr + ni * li) / den
    coef_im = (ni * lr - nr * li) / den
    br, bi = b_re.astype(jnp.float32), b_im.astype(jnp.float32)
    bb_re = coef_re[..., None] * br - coef_im[..., None] * bi
    bb_im = coef_re[..., None] * bi + coef_im[..., None] * br
    bu_re = jnp.einsum('bsgc,gnc->bsgn', uf, bb_re)
    bu_im = jnp.einsum('bsgc,gnc->bsgn', uf, bb_im)
    a_re = jnp.broadcast_to(lb_re, bu_re.shape)
    a_im = jnp.broadcast_to(lb_im, bu_im.shape)
    _, _, x_re, x_im = lax.associative_scan(
        complex_linear_combine, (a_re, a_im, bu_re, bu_im), axis=1)
    y = (jnp.einsum('bsgn,gcn->bsgc', x_re, c_re.astype(jnp.float32))
         - jnp.einsum('bsgn,gcn->bsgc', x_im, c_im.astype(jnp.float32))
         + d_skip.astype(jnp.float32) * uf)
    z = jax.nn.gelu(y.reshape(bsz, s, S5_WIDTH)).astype(u.dtype)
    return z * jax.nn.sigmoid(z @ glu_w + glu_b)


def hier_moe(h, rg_w, rg_b, re_w, re_b, w_gate, w_up, w_down):
    bsz, s, dm = h.shape
    t = h.reshape(-1, dm)
    tn = t.shape[0]
    g_prob = jax.nn.softmax((t @ rg_w + rg_b).astype(jnp.float32), axis=-1)
    g_p, g_idx = lax.top_k(g_prob, 1)
    e_logits = (t @ re_w + re_b).astype(jnp.float32).reshape(tn, N_GROUPS, EXPERTS_PER_GROUP)
    e_in_group = jnp.take_along_axis(e_logits, g_idx[:, :, None], axis=1)[:, 0]
    e_top, e_idx = lax.top_k(e_in_group, TOP_K)
    e_w = jax.nn.softmax(e_top, axis=-1) * g_p
    expert_id = g_idx * EXPERTS_PER_GROUP + e_idx
    comb = jnp.sum(jax.nn.one_hot(expert_id, N_EXPERTS, dtype=jnp.float32) * e_w[..., None],
                   axis=1)
    out = jnp.zeros((tn, dm), jnp.float32)
    for e in range(N_EXPERTS):
        hid = jax.nn.silu(t @ w_gate[e]) * (t @ w_up[e])
        out = out + comb[:, e:e + 1] * (hid @ w_down[e]).astype(jnp.float32)
    return out.reshape(bsz, s, dm).astype(h.dtype)


def setup_inputs(seed: int = 0) -> dict:
    key = jax.random.key(seed)
    ks = iter(jax.random.split(key, 48))
    L, D = DEPTH, D_MODEL
    nrm = lambda shape, std: std * jax.random.normal(next(ks), shape, jnp.float32)
    unif = lambda shape, lo, hi: jax.random.uniform(next(ks), shape, jnp.float32, lo, hi)
    n_idx = jnp.arange(S5_STATE, dtype=jnp.float32)
    return {
        "x": nrm((BATCH, SEQ, D), 1.0),
        "p": nrm((L, BATCH, SEQ, PLE_DIM), 1.0),
        "mix_norm": 1.0 + nrm((L, D), 0.02),
        "w_in": nrm((L, D, IN_COLS), D ** -0.5),
        "mu_shift": unif((L, RWKV_COLS), 0.0, 1.0),
        "rk_w0": unif((L, RWKV_WIDTH), -6.0, 1.0),
        "rk_w_up": nrm((L, DECAY_LORA, RWKV_WIDTH), DECAY_LORA ** -0.5),
        "rk_a0": nrm((L, RWKV_WIDTH), 0.1),
        "rk_a_up": nrm((L, AAA_LORA, RWKV_WIDTH), AAA_LORA ** -0.5),
        "rk_g_up": nrm((L, GATE_LORA, RWKV_WIDTH), GATE_LORA ** -0.5),
        "rk_k_k": 0.85 + nrm((L, RWKV_WIDTH), 0.02),
        "rk_k_a": 1.0 + nrm((L, RWKV_WIDTH), 0.02),
        "rk_r_k": nrm((L, RWKV_HEADS, RWKV_HEAD_DIM), 0.1),
        "rk_ln_g": 1.0 + nrm((L, RWKV_WIDTH), 0.02),
        "rk_ln_b": nrm((L, RWKV_WIDTH), 0.02),
        "s5_lam_re": -0.5 + nrm((L, S5_GROUPS, S5_STATE), 0.01),
        "s5_lam_im": math.pi * n_idx + nrm((L, S5_GROUPS, S5_STATE), 0.01),
        "s5_log_dt": unif((L, S5_GROUPS), math.log(1e-3), math.log(1e-1)),
        "s5_b_re": nrm((L, S5_GROUPS, S5_STATE, S5_GROUP_CH), (0.5 / S5_GROUP_CH) ** 0.5),
        "s5_b_im": nrm((L, S5_GROUPS, S5_STATE, S5_GROUP_CH), (0.5 / S5_GROUP_CH) ** 0.5),
        "s5_c_re": nrm((L, S5_GROUPS, S5_GROUP_CH, S5_STATE), 0.5 ** 0.5),
        "s5_c_im": nrm((L, S5_GROUPS, S5_GROUP_CH, S5_STATE), 0.5 ** 0.5),
        "s5_d": nrm((L, S5_GROUPS, S5_GROUP_CH), 1.0),
        "s5_glu_w": nrm((L, S5_WIDTH, S5_WIDTH), S5_WIDTH ** -0.5),
        "s5_glu_b": nrm((L, S5_WIDTH), 0.02),
        "w_branch_a": nrm((L, RWKV_WIDTH, D), RWKV_WIDTH ** -0.5),
        "w_branch_b": nrm((L, S5_WIDTH, D), S5_WIDTH ** -0.5),
        "w_out": nrm((L, D, D), D ** -0.5),
        "ffn_norm": 1.0 + nrm((L, D), 0.02),
        "router_group_w": nrm((L, D, N_GROUPS), D ** -0.5),
        "router_group_b": nrm((L, N_GROUPS), 0.01),
        "router_expert_w": nrm((L, D, N_EXPERTS), D ** -0.5),
        "router_expert_b": nrm((L, N_EXPERTS), 0.01),
        "exp_w_gate": nrm((L, N_EXPERTS, D, D_EXPERT), D ** -0.5),
        "exp_w_up": nrm((L, N_EXPERTS, D, D_EXPERT), D ** -0.5),
        "exp_w_down": nrm((L, N_EXPERTS, D_EXPERT, D), D_EXPERT ** -0.5),
        "ple_norm": 1.0 + nrm((L, D), 0.02),
        "ple_gate_w": nrm((L, D, D), D ** -0.5),
        "ple_proj": nrm((L, PLE_DIM, D), PLE_DIM ** -0.5),
        "final_norm": 1.0 + nrm((D,), 0.02),
    }


def reference(x, p, mix_norm, w_in, mu_shift, rk_w0, rk_w_up, rk_a0, rk_a_up, rk_g_up,
              rk_k_k, rk_k_a, rk_r_k, rk_ln_g, rk_ln_b, s5_lam_re, s5_lam_im, s5_log_dt,
              s5_b_re, s5_b_im, s5_c_re, s5_c_im, s5_d, s5_glu_w, s5_glu_b,
              w_branch_a, w_branch_b, w_out, ffn_norm, router_group_w, router_group_b,
              router_expert_w, router_expert_b, exp_w_gate, exp_w_up, exp_w_down,
              ple_norm, ple_gate_w, ple_proj, final_norm):
    for i in range(DEPTH):
        h = rms_norm(x, mix_norm[i])
        cols = h @ w_in[i]
        c_rwkv, u_s5, gate_a, gate_b = jnp.split(
            cols, [RWKV_COLS, RWKV_COLS + S5_WIDTH, RWKV_COLS + S5_WIDTH + D_MODEL], axis=-1)
        prev = jnp.pad(c_rwkv, ((0, 0), (1, 0), (0, 0)))[:, :-1]
        c_rwkv = c_rwkv + (prev - c_rwkv) * mu_shift[i]
        y_a = rwkv7_branch(c_rwkv, rk_w0[i], rk_w_up[i], rk_a0[i], rk_a_up[i], rk_g_up[i],
                           rk_k_k[i], rk_k_a[i], rk_r_k[i], rk_ln_g[i], rk_ln_b[i]) @ w_branch_a[i]
        y_b = s5_branch(u_s5, s5_lam_re[i], s5_lam_im[i], s5_log_dt[i], s5_b_re[i], s5_b_im[i],
                        s5_c_re[i], s5_c_im[i], s5_d[i], s5_glu_w[i], s5_glu_b[i]) @ w_branch_b[i]
        merged = jax.nn.sigmoid(gate_a) * y_a + jax.nn.sigmoid(gate_b) * y_b
        x = x + merged @ w_out[i]
        x = x + hier_moe(rms_norm(x, ffn_norm[i]), router_group_w[i], router_group_b[i],
                         router_expert_w[i], router_expert_b[i],
                         exp_w_gate[i], exp_w_up[i], exp_w_down[i])
        hp = rms_norm(x, ple_norm[i])
        x = x + jax.nn.sigmoid(hp @ ple_gate_w[i]) * (p[i] @ ple_proj[i])
    return rms_norm(x, final_norm)
```

```python
import contextlib
import math
import numpy as np
import concourse.bass as bass
import concourse.mybir as mybir
from concourse.bass_utils import run_bass_kernel_spmd

F32 = mybir.dt.float32
BF16 = mybir.dt.bfloat16
AF = mybir.ActivationFunctionType
ALU = mybir.AluOpType
AX = mybir.AxisListType

ENGS = ["tensor", "vector", "scalar", "gpsimd", "sync"]
D = 1024
T = 512
NCORES = 8
CDEC = math.exp(-0.5)

PV = {}
_o = 0
for _n, _w in [("mix", 8), ("ffn", 8), ("ple", 8), ("fin", 8), ("mu", 14), ("w0", 4), ("a0", 4),
               ("kk", 4), ("ka", 4), ("rk", 4), ("lng", 4), ("lnb", 4), ("s5d", 2), ("glub", 2),
               ("lre", 8), ("lim", 8), ("ldt", 8)]:
    PV[_n] = _o
    _o += _w
NPV = _o


class Buf:
    __slots__ = ("name", "t", "last_w", "readers", "dma_sem", "dma_cnt", "aliases", "root")

    def __init__(self, name, t):
        self.name = name
        self.t = t
        self.last_w = None
        self.readers = []
        self.dma_sem = None
        self.dma_cnt = 0
        self.aliases = []
        self.root = None

    def __getitem__(self, k):
        return self.t[k]


class Prog:
    def __init__(self, nc):
        self.nc = nc
        self.root = contextlib.ExitStack()
        self.stacks = [self.root]
        self.scope_bufs = [[]]
        self.ops = {e: [] for e in ENGS}
        self.esem = {}
        self.ecnt = {e: 0 for e in ENGS}
        self.dma_bufs = []
        self.barrier_bufs = []
        for e in ENGS:
            self.esem[e] = self.root.enter_context(nc.semaphore("es_" + e))
        self.dbg = []
        self.waited = {}
        self.stall_fill = True
        self.side_budget = 0
        self.side = None
        self.tick_every = 8
        self._tick_cnt = 0
        self._in_side = False

    def _tick(self, force=False):
        if self.side is None or self._in_side:
            return
        if self.side_budget <= 0:
            return
        if not force:
            self._tick_cnt += 1
            if self._tick_cnt % self.tick_every:
                return
        self.side_budget -= 1
        self._in_side = True
        try:
            next(self.side)
        except StopIteration:
            self.side = None
        finally:
            self._in_side = False

    def drain_side(self):
        if self.side is None:
            return
        self._in_side = True
        try:
            for _ in self.side:
                pass
        finally:
            self._in_side = False
            self.side = None

    def sb(self, name, shape, dtype=F32):
        self.nuid = getattr(self, "nuid", 0) + 1
        name = "s%d_%s" % (self.nuid, name)
        t = self.stacks[-1].enter_context(self.nc.sbuf_tensor(name, list(shape), dtype))
        b = Buf(name, t)
        self.scope_bufs[-1].append(b)
        return b

    def ps_bank(self, name):
        t = self.root.enter_context(self.nc.psum_tensor(name, [128, 512], F32))
        return Buf(name, t)

    def view(self, parent, name, ap):
        b = Buf(name, ap)
        b.aliases.append(parent)
        parent.aliases.append(b)
        return b

    @contextlib.contextmanager
    def scope(self):
        st = contextlib.ExitStack()
        self.stacks.append(st)
        self.scope_bufs.append([])
        try:
            yield
        finally:
            self.barrier()
            self.stacks.pop()
            self.scope_bufs.pop()
            st.close()

    def barrier(self):
        for e in ENGS:
            waits = []
            for o in ENGS:
                if o != e and o != "sync" and self.ecnt[o] > 0:
                    waits.append((self.esem[o], self.ecnt[o]))
            for b in self.barrier_bufs:
                if b.dma_cnt > 0:
                    waits.append((b.dma_sem, b.dma_cnt))
            self.ops[e].append((None, waits, None))

    def _dma_sem(self, b):
        if b.dma_sem is None:
            b.dma_sem = self.root.enter_context(self.nc.semaphore("ds_" + b.name))
            self.dma_bufs.append(b)
        return b.dma_sem

    def _waits(self, eng, reads, writes, dry=False):
        w = {}

        def need(dep):
            if dep is None:
                return
            kind, key, val = dep
            if kind == "eng":
                if key == eng and eng == "tensor":
                    return
                k = ("eng", key)
                sem = self.esem[key]
            else:
                k = ("dma", id(key))
                sem = key.dma_sem
            if w.get(k, (None, 0))[1] < val:
                w[k] = (sem, val)

        for b0 in reads:
            for b in [b0] + b0.aliases:
                need(b.last_w)
        for b0 in writes:
            for b in [b0] + b0.aliases:
                need(b.last_w)
                for r in b.readers:
                    need(r)
        q = eng.split("_")[-1]
        wd = self.waited.setdefault(q, {})
        out = []
        for kk_, (sem, val) in w.items():
            if wd.get(kk_, 0) >= val:
                continue
            if not dry:
                wd[kk_] = val
            out.append((sem, val))
        return out

    def op_silent(self, eng, fn, reads=(), wwait=()):
        assert eng == "tensor"
        reads = [b.root if b.root is not None else b for b in reads]
        wwait = [b.root if b.root is not None else b for b in wwait]
        if self.side is not None and not self._in_side and self.stall_fill:
            if self._waits(eng, reads, wwait, dry=True):
                self._tick(force=True)
        waits = self._waits(eng, reads, wwait)
        self.ops[eng].append((fn, waits, None))

    def op(self, eng, fn, reads=(), writes=()):
        reads = [b.root if b.root is not None else b for b in reads]
        writes = [b.root if b.root is not None else b for b in writes]
        if eng == "tensor" and self.side is not None and not self._in_side and self.stall_fill:
            if self._waits(eng, reads, writes, dry=True):
                self._tick(force=True)
        waits = self._waits(eng, reads, writes)
        self.ecnt[eng] += 1
        val = self.ecnt[eng]
        self.ops[eng].append((fn, waits, (self.esem[eng], 1)))
        dep = ("eng", eng, val)
        for b in reads:
            b.readers.append(dep)
            if len(b.readers) > 16:
                mx = {}
                for r in b.readers:
                    k = (r[0], id(r[1]) if r[0] == "dma" else r[1])
                    if k not in mx or mx[k][2] < r[2]:
                        mx[k] = r
                b.readers = list(mx.values())
        for b in writes:
            b.last_w = dep
            b.readers = []
        self._tick()
        return val

    def dma(self, eng, out_ap, in_ap, reads=(), writes=(), sem_buf=None):
        waits = self._waits("dmaq_" + eng, reads, writes)
        sb_ = sem_buf if sem_buf is not None else writes[0]
        sem = self._dma_sem(sb_)
        sb_.dma_cnt += 16
        val = sb_.dma_cnt

        def fn(e, out_ap=out_ap, in_ap=in_ap):
            return e.dma_start(out=out_ap, in_=in_ap)
        self.ops[eng].append((fn, waits, (sem, 16)))
        dep = ("dma", sb_, val)
        for b in reads:
            b.readers.append(dep)
        for b in writes:
            b.last_w = dep
            b.readers = []
        if sem_buf is not None and not writes:
            sem_buf.last_w = dep

    def final_wait(self, eng, bufs):
        waits = self._waits("final_" + eng, bufs, ())
        self.ops[eng].append((None, waits, None))

    def emit(self):
        with self.nc.Block() as block:
            for e in ENGS:
                ops = self.ops[e]
                if not ops:
                    continue

                def body(eng_obj, ops=ops):
                    for fn, waits, inc in ops:
                        for sem, val in waits:
                            eng_obj.wait_ge(sem, val)
                        if fn is None:
                            continue
                        ins = fn(eng_obj)
                        if inc is not None:
                            ins.then_inc(inc[0], inc[1])
                getattr(block, e)(body)


class Builder:
    def __init__(self, nseq, seq, en_rwkv=True, en_s5=True, en_moe=True, en_ple=True, dbg=()):
        self.nseq, self.seq = nseq, seq
        self.en_rwkv, self.en_s5, self.en_moe, self.en_ple = en_rwkv, en_s5, en_moe, en_ple
        self.dbg_names = set(dbg)
        self.nc = bass.Bass("TRN2", target_bir_lowering=False)
        self.p = Prog(self.nc)
        self.dbg_outs = {}
        self._rr = 0
        self._cnt = 0
        self._grp_reads = {}
        self.silent_mm = True
        self.tick_rwkv, self.tick_s5, self.tick_merge = 100000, 6, 8
        self.bud_rwkv, self.bud_s5, self.bud_merge = 1000, 1000, 1000

    def uid(self, s):
        self._cnt += 1
        return "%s_%d" % (s, self._cnt)

    def din(self, name, shape, dtype=F32):
        return self.nc.dram_tensor(name, list(shape), dtype, kind="ExternalInput").ap()

    def V(self, fn, reads, writes):
        self.p.op("vector", fn, reads, writes)

    def A(self, fn, reads, writes):
        self.p.op("scalar", fn, reads, writes)

    def G(self, fn, reads, writes):
        self.p.op("gpsimd", fn, reads, writes)

    def MM(self, out_ap, lhsT, rhs, start, stop, reads, writes):
        key = id(writes[0].root if writes[0].root is not None else writes[0])
        if start:
            self._grp_reads = {}
        pend = self._grp_reads.setdefault(key, [])
        if not stop and self.silent_mm:
            if start:
                self.p.op_silent("tensor", lambda e: e.matmul(out_ap, lhsT=lhsT, rhs=rhs, start=start, stop=stop), reads, writes)
            else:
                self.p.op_silent("tensor", lambda e: e.matmul(out_ap, lhsT=lhsT, rhs=rhs, start=start, stop=stop), reads)
            pend.extend(reads)
            return
        allr = list(reads) + [b for b in pend if b not in reads]
        self._grp_reads[key] = []
        self.p.op("tensor", lambda e: e.matmul(out_ap, lhsT=lhsT, rhs=rhs, start=start, stop=stop), allr, writes)

    def act(self, out_b, out_ap, in_b, in_ap, func, bias=None, scale=None, extra_reads=()):
        kw = {}
        if bias is not None:
            kw["bias"] = bias
        if scale is not None:
            kw["scale"] = scale
        self.A(lambda e: e.activation(out=out_ap, in_=in_ap, func=func, **kw), [in_b] + list(extra_reads), [out_b])

    def next_ps(self):
        b = self.psr[self._rr % len(self.psr)]
        self._rr += 1
        return b

    def debug_out(self, name, buf, ap, shape, dtype=F32):
        if name not in self.dbg_names:
            return
        d = self.nc.dram_tensor("dbg_" + name, list(shape), dtype, kind="ExternalOutput").ap()
        db = Buf("dbg_" + name, d)
        self.p.dma("sync", d, ap, reads=[buf], writes=[db])
        self.p.barrier_bufs.append(db)
        self.dbg_outs[name] = db

    def load_w(self, dram_ap, kt, cols, key):
        n = kt * cols
        assert n <= 2048
        if self.p._in_side:
            i = self.NMAIN + (self._wrr_side % (len(self.wbf) - self.NMAIN))
            self._wrr_side += 1
        else:
            i = self._wrr % self.NMAIN
            self._wrr += 1
        wb = self.wbf[i]
        wbv = wb[:, 0:n].rearrange("p (k c) -> p k c", k=kt)
        if key in self.wcache:
            off = self.wcache[key]
            self.p.dma("sync", wb[:, 0:n], self.wscr[:, off:off + n], reads=([] if self.wscr_ready else self.wsb), writes=[wb])
            return wb, wbv
        assert self.wst is not None, "first use of %s after staging was freed" % (key,)
        st = self.wst[self._srr % len(self.wst)]
        self._srr += 1
        stv = st[:, 0:n].rearrange("p (k c) -> p k c", k=kt)
        self.p.dma("sync", stv, dram_ap.rearrange("(k p) c -> p k c", p=128), writes=[st])
        ce = self._crr % 3
        self._crr += 1
        if ce == 0:
            self.G(lambda e: e.tensor_copy(out=wb[:, 0:n], in_=st[:, 0:n]), [st], [wb])
        elif ce == 1:
            self.A(lambda e: e.activation(out=wb[:, 0:n], in_=st[:, 0:n], func=AF.Copy), [st], [wb])
        else:
            self.V(lambda e: e.tensor_copy(out=wb[:, 0:n], in_=st[:, 0:n]), [st], [wb])
        off = self.wscr_off
        self.wscr_off += n
        assert self.wscr_off <= self.WSCR_COLS
        self.wcache[key] = off
        self.p.dma("sync", self.wscr[:, off:off + n], wb[:, 0:n], reads=[wb], writes=[], sem_buf=self.wsb[i])
        return wb, wbv

    def rmsnorm(self, x, gcol, out_b, out_f=None, out_is_f32_only=False):
        p = self.p
        ps = self.next_ps()
        sil_, self.silent_mm = self.silent_mm, False
        for k in range(8):
            sq = self.sqb[k % 2]
            self.act(sq, sq[:], x, x[:, k, :], AF.Square)
            self.MM(ps[:], self.ones_bf[:], sq[:], k == 0, k == 7, [self.ones_bf, sq], [ps])
        self.silent_mm = sil_
        rt = self.rt
        self.act(rt, rt[:], ps, ps[:], AF.Sqrt, bias=self.eps_col[:, 0:1], scale=1.0 / D, extra_reads=[self.eps_col])
        self.V(lambda e: e.reciprocal(out=rt[:], in_=rt[:]), [rt], [rt])
        pv = self.pvec
        for k in range(8):
            if out_f is not None:
                self.V(lambda e, k=k: e.scalar_tensor_tensor(out=out_f[:, k, :], in0=x[:, k, :], scalar=pv[:, gcol + k:gcol + k + 1],
                                                             in1=rt[:], op0=ALU.mult, op1=ALU.mult), [x, pv, rt], [out_f])
                if out_b is not None:
                    self.G(lambda e, k=k: e.tensor_copy(out=out_b[:, k, :], in_=out_f[:, k, :]), [out_f], [out_b])
            else:
                self.V(lambda e, k=k: e.scalar_tensor_tensor(out=out_b[:, k, :], in0=x[:, k, :], scalar=pv[:, gcol + k:gcol + k + 1],
                                                             in1=rt[:], op0=ALU.mult, op1=ALU.mult), [x, pv, rt], [out_b])

    def build(self):
        nc, p = self.nc, self.p
        nseq, seq = self.nseq, self.seq
        nchunk = seq // T
        self.xT = self.din("xT", [nseq, D, seq])
        self.pT = self.din("pT", [nseq, 256, seq])
        self.pvec_d = self.din("pvec", [128, NPV])
        self.w_in = self.din("w_in", [D, 4096])
        self.lora_wa = self.din("lora_wa", [128, 512])
        self.lora_g = self.din("lora_g", [128, 512])
        self.glu_w = self.din("glu_w", [256, 256])
        self.w_ba = self.din("w_ba", [512, D])
        self.w_bb = self.din("w_bb", [256, D])
        self.w_out = self.din("w_out", [D, D])
        self.wr = self.din("wr", [D, 36])
        self.rbias_d = self.din("rbias", [128, 36])
        self.wg = self.din("wg", [32, D, 256])
        self.wu = self.din("wu", [32, D, 256])
        self.wd = self.din("wd", [32, 256, D])
        self.plg = self.din("plg", [D, D])
        self.plp = self.din("plp", [256, D])
        self.s5bre = self.din("s5bre", [256, 512])
        self.s5bim = self.din("s5bim", [256, 512])
        self.s5cre = self.din("s5cre", [128, 8 * 128])
        self.s5cim = self.din("s5cim", [128, 8 * 128])
        self.outT = self.nc.dram_tensor("outT", [nseq, D, seq], F32, kind="ExternalOutput").ap()
        self.out_b = Buf("outT", self.outT)
        p.barrier_bufs.append(self.out_b)

        self.banks = [p.ps_bank("bank%d" % i) for i in range(8)]
        self.psr = self.banks[0:4]
        self.psa = self.banks[4:6]
        self.ps_side = self.banks[6:8]

        self.pvec = p.sb("pvec", [128, NPV])
        self.xs = [p.sb("x0", [128, 8, T])]
        self.x = self.xs[0]
        self.hb = p.sb("hb", [128, 8, T], BF16)
        self.hbm = p.sb("hbm", [128, 8, T], BF16)
        self.sqb = [p.sb("sq%d" % i, [128, T], BF16) for i in range(2)]
        self.rt = p.sb("rt", [128, T])
        self.ones_bf = p.sb("ones_bf", [128, 128], BF16)
        self.eps_col = p.sb("eps_col", [128, 4])
        self.ident_f = p.sb("ident_f", [128, 128])
        self.ident_b = p.sb("ident_b", [128, 128], BF16)
        self.wst = None
        self.wbf = [p.sb("wbf%d" % i, [128, 2048], BF16) for i in range(9)]
        self._wrr = 0
        self._wrr_side = 0
        self.NMAIN = 4
        self._srr = 0
        self._crr = 0
        self.wcache = {}
        self.wscr_off = 0
        self.WSCR_COLS = 262144
        self.wscr = self.nc.dram_tensor("wscr", [128, self.WSCR_COLS], BF16, kind="Internal").ap()
        self.wsb = [Buf("wsb%d" % i, self.wscr) for i in range(len(self.wbf))]
        self.wscr_ready = False
        self.wr_sb = p.sb("wr_sb", [128, 8, 36])
        self.rbias = p.sb("rbias", [128, 36])
        self.combT = p.sb("combT", [32, T], BF16)
        self.m_cb = [p.sb("cb%d" % i, [128, T], BF16) for i in range(2)]
        self.m_sg = [p.sb("sg%d" % i, [128, T], BF16) for i in range(2)]
        self.m_tu = [p.sb("tu%d" % i, [128, T], BF16) for i in range(2)]
        self.m_hid = [p.sb("hid%d" % i, [128, 2, T], BF16) for i in range(4)]

        p.dma("sync", self.pvec[:], self.pvec_d, writes=[self.pvec])
        p.dma("sync", self.wr_sb[:], self.wr.rearrange("(k p) c -> p k c", p=128), writes=[self.wr_sb])
        p.dma("sync", self.rbias[:], self.rbias_d, writes=[self.rbias])
        self.G(lambda e: e.memset(self.ones_bf[:], 1.0), [], [self.ones_bf])
        self.G(lambda e: e.memset(self.eps_col[:, 0:1], 1e-6), [], [self.eps_col])
        self.G(lambda e: e.memset(self.eps_col[:, 1:2], 64e-5), [], [self.eps_col])
        self.G(lambda e: e.memset(self.eps_col[:, 2:3], math.pi / 2), [], [self.eps_col])
        self.G(lambda e: e.memset(self.eps_col[:, 3:4], 0.0), [], [self.eps_col])
        self.G(lambda e: e.memset(self.ident_f[:], 1.0), [], [self.ident_f])
        self.G(lambda e: e.affine_select(out=self.ident_f[:], in_=self.ident_f[:], pattern=[[-1, 128]],
                                         compare_op=ALU.is_equal, fill=0.0, base=0, channel_multiplier=1),
               [self.ident_f], [self.ident_f])
        self.G(lambda e: e.tensor_copy(out=self.ident_b[:], in_=self.ident_f[:]), [self.ident_f], [self.ident_b])

        if self.en_rwkv:
            self.setup_rwkv(0)
        if self.en_s5:
            self.setup_s5(0)

        units = [(s, c) for c in range(nchunk) for s in range(nseq)]

        def load_x(ui):
            s, c = units[ui]
            x = self.xs[ui % 2]
            p.dma("sync", x[:], self.xT[s, :, c * T:(c + 1) * T].rearrange("(k p) t -> p k t", p=128), writes=[x])
            return x

        def do_mixer(ui, x):
            s, c = units[ui]
            self.x = x
            if self.en_rwkv or self.en_s5:
                self.set_seq(s)
                with p.scope():
                    self.mixer(s, c)
            if ui == len(units) - 1:
                self.debug_out("x_mix", x, x[:], [128, 8, T])

        def do_tail(ui, x):
            s, c = units[ui]
            if ui == len(units) - 1:
                self.debug_out("x_moe", x, x[:], [128, 8, T])
            with p.scope():
                if self.en_ple:
                    self.ple(x, s, c)
                self.final(x, s, c, ui == len(units) - 1)

        with p.scope():
            self.wst = [p.sb("wst%d" % i, [128, 2048]) for i in range(2)]
            if self.en_rwkv:
                self.setup_rwkv(1)
            if self.en_s5:
                self.setup_s5(1)
            x0 = load_x(0)
            do_mixer(0, x0)
            if self.en_moe:
                with p.scope():
                    self.moe_head(x0)
                with p.scope():
                    wst0 = self.wst
                    self.wst = wst0 + [p.sb("wstx%d" % i, [128, 2048]) for i in range(6)]
                    nmain0, self.NMAIN = self.NMAIN, len(self.wbf)
                    for _ in self.experts_gen(x0, self.banks):
                        pass
                    self.NMAIN = nmain0
                    self._wrr = 0
                    self.wst = wst0
            do_tail(0, x0)
            p.final_wait("sync", self.wsb)
            self.wscr_ready = True
        self.wst = None
        self.xs.append(p.sb("x1", [128, 8, T]))
        prev = None
        for ui in range(1, len(units)):
            x = load_x(ui)
            if prev is not None and self.en_moe:
                p.side = self.experts_gen(prev[1], self.ps_side)
            do_mixer(ui, x)
            p.drain_side()
            if prev is not None:
                do_tail(prev[0], prev[1])
            if self.en_moe:
                with p.scope():
                    self.moe_head(x)
            prev = (ui, x)
        if prev is not None:
            if self.en_moe:
                for _ in self.experts_gen(prev[1], self.banks):
                    pass
            do_tail(prev[0], prev[1])

        p.final_wait("sync", [self.out_b] + list(self.dbg_outs.values()))
        p.emit()
        return nc

    def set_seq(self, s):
        pass

    def moe_head(self, x):
        p = self.p
        pv = self.pvec
        h2f = p.sb("h2f", [128, 8, T])
        hb = self.hbm
        self.rmsnorm(x, PV["ffn"], hb, out_f=h2f)
        lg = p.sb("lg", [128, 4, 36])
        sm4 = p.sb("sm4", [128, 8, 4])
        gm = p.sb("gm", [128, 4, 4])
        ge = p.sb("ge", [128, 4, 4])
        el = p.sb("el", [128, 4, 8])
        el2 = p.sb("el2", [128, 4, 8])
        t8 = p.sb("t8", [128, 4, 8])
        m1 = p.sb("m1", [128, 4, 8])
        m2 = p.sb("m2", [128, 4, 8])
        cg = p.sb("cg", [128, 4, 8])
        comb = p.sb("comb", [128, 4, 32])
        combT = self.combT
        S = lambda i: sm4[:, i, :]
        Sb = lambda i, n: sm4[:, i, :].unsqueeze(2).to_broadcast([128, 4, n])
        for tb in range(4):
            ps = self.next_ps()
            for k in range(8):
                self.MM(ps[:, 0:36], h2f[:, k, tb * 128:(tb + 1) * 128], self.wr_sb[:, k, :], k == 0, k == 7, [h2f, self.wr_sb], [ps])
            self.V(lambda e, ps=ps, tb=tb: e.tensor_tensor(out=lg[:, tb, :], in0=ps[:, 0:36], in1=self.rbias[:], op=ALU.add), [ps, self.rbias], [lg])
        V = self.V
        V(lambda e: e.tensor_reduce(out=S(0), in_=lg[:, :, 0:4], axis=AX.X, op=ALU.max), [lg], [sm4])
        V(lambda e: e.tensor_tensor(out=gm[:], in0=lg[:, :, 0:4], in1=Sb(0, 4), op=ALU.is_equal), [lg, sm4], [gm])
        V(lambda e: e.tensor_tensor(out=ge[:], in0=lg[:, :, 0:4], in1=Sb(0, 4), op=ALU.subtract), [lg, sm4], [ge])
        self.A(lambda e: e.activation(out=ge[:], in_=ge[:], func=AF.Exp), [ge], [ge])
        V(lambda e: e.tensor_reduce(out=S(1), in_=ge[:], axis=AX.X, op=ALU.add), [ge], [sm4])
        V(lambda e: e.reciprocal(out=S(2), in_=S(1)), [sm4], [sm4])
        V(lambda e: e.tensor_tensor(out=el[:], in0=lg[:, :, 4:12], in1=gm[:, :, 0:1].to_broadcast([128, 4, 8]), op=ALU.mult), [lg, gm], [el])
        for g in range(1, 4):
            V(lambda e, g=g: e.tensor_tensor(out=t8[:], in0=lg[:, :, 4 + 8 * g:12 + 8 * g], in1=gm[:, :, g:g + 1].to_broadcast([128, 4, 8]), op=ALU.mult), [lg, gm], [t8])
            V(lambda e: e.tensor_tensor(out=el[:], in0=el[:], in1=t8[:], op=ALU.add), [el, t8], [el])
        V(lambda e: e.tensor_reduce(out=S(3), in_=el[:], axis=AX.X, op=ALU.max), [el], [sm4])
        V(lambda e: e.tensor_tensor(out=m1[:], in0=el[:], in1=Sb(3, 8), op=ALU.is_equal), [el, sm4], [m1])
        V(lambda e: e.scalar_tensor_tensor(out=el2[:], in0=m1[:], scalar=-1e30, in1=el[:], op0=ALU.mult, op1=ALU.add), [m1, el], [el2])
        V(lambda e: e.tensor_reduce(out=S(4), in_=el2[:], axis=AX.X, op=ALU.max), [el2], [sm4])
        V(lambda e: e.tensor_tensor(out=m2[:], in0=el2[:], in1=Sb(4, 8), op=ALU.is_equal), [el2, sm4], [m2])
        V(lambda e: e.tensor_tensor(out=S(5), in0=S(3), in1=S(4), op=ALU.subtract), [sm4], [sm4])
        self.A(lambda e: e.activation(out=S(5), in_=S(5), func=AF.Sigmoid), [sm4], [sm4])
        V(lambda e: e.tensor_scalar(out=S(6), in0=S(5), scalar1=-1.0, scalar2=1.0, op0=ALU.mult, op1=ALU.add), [sm4], [sm4])
        V(lambda e: e.tensor_tensor(out=S(5), in0=S(5), in1=S(2), op=ALU.mult), [sm4], [sm4])
        V(lambda e: e.tensor_tensor(out=S(6), in0=S(6), in1=S(2), op=ALU.mult), [sm4], [sm4])
        V(lambda e: e.tensor_tensor(out=cg[:], in0=m1[:], in1=Sb(5, 8), op=ALU.mult), [m1, sm4], [cg])
        V(lambda e: e.tensor_tensor(out=t8[:], in0=m2[:], in1=Sb(6, 8), op=ALU.mult), [m2, sm4], [t8])
        V(lambda e: e.tensor_tensor(out=cg[:], in0=cg[:], in1=t8[:], op=ALU.add), [cg, t8], [cg])
        for g in range(4):
            V(lambda e, g=g: e.tensor_tensor(out=comb[:, :, 8 * g:8 * g + 8], in0=cg[:], in1=gm[:, :, g:g + 1].to_broadcast([128, 4, 8]), op=ALU.mult), [cg, gm], [comb])
        ps2 = self.next_ps()
        sil_, self.silent_mm = self.silent_mm, False
        for tb in range(4):
            self.MM(ps2[0:32, tb * 128:(tb + 1) * 128], comb[:, tb, :], self.ident_f[:], True, True, [comb, self.ident_f], [ps2])
        self.silent_mm = sil_
        self.A(lambda e, ps2=ps2: e.activation(out=combT[:], in_=ps2[0:32, :], func=AF.Copy), [ps2], [combT])

    def experts_gen(self, x, banks):
        p = self.p
        hb, combT = self.hbm, self.combT
        cb, sg, tu, hid = self.m_cb, self.m_sg, self.m_tu, self.m_hid
        st = {"rr": 0}

        def nps():
            b = banks[st["rr"] % len(banks)]
            st["rr"] += 1
            return b
        for pr in range(16):
            hds = []
            for el_ in range(2):
                ex = pr * 2 + el_
                psc = nps()
                self.MM(psc[:], self.ident_b[0:32, ex:ex + 1].to_broadcast([32, 128]), combT[:], True, True, [self.ident_b, combT], [psc])
                cbe = cb[ex % 2]
                self.act(cbe, cbe[:], psc, psc[:], AF.Copy)
                wgb, wgv = self.load_w(self.wg[ex], 8, 256, ("wg", ex))
                wub, wuv = self.load_w(self.wu[ex], 8, 256, ("wu", ex))
                hd = hid[ex % 4]
                hds.append(hd)
                yield
                for blk in range(2):
                    pg = nps()
                    for k in range(8):
                        self.MM(pg[:], wgv[:, k, blk * 128:(blk + 1) * 128], hb[:, k, :], k == 0, k == 7, [wgb, hb], [pg])
                    sgb, tub = sg[blk], tu[blk]
                    self.act(sgb, sgb[:], pg, pg[:], AF.Silu)
                    yield
                    pu = nps()
                    for k in range(8):
                        self.MM(pu[:], wuv[:, k, blk * 128:(blk + 1) * 128], hb[:, k, :], k == 0, k == 7, [wub, hb], [pu])
                    self.V(lambda e, pu=pu, sgb=sgb, tub=tub: e.tensor_tensor(out=tub[:], in0=pu[:], in1=sgb[:], op=ALU.mult), [pu, sgb], [tub])
                    self.G(lambda e, tub=tub, cbe=cbe, hd=hd, blk=blk: e.tensor_tensor(out=hd[:, blk, :], in0=tub[:], in1=cbe[:], op=ALU.mult), [tub, cbe], [hd])
                    yield
            wds = [self.load_w(self.wd[pr * 2 + el_], 2, 1024, ("wd", pr * 2 + el_)) for el_ in range(2)]
            for db in range(8):
                pd = nps()
                for el_ in range(2):
                    wdb, wdv = wds[el_]
                    for blk in range(2):
                        self.MM(pd[:], wdv[:, blk, db * 128:(db + 1) * 128], hds[el_][:, blk, :], el_ == 0 and blk == 0, el_ == 1 and blk == 1, [wdb, hds[el_]], [pd])
                self.V(lambda e, pd=pd, db=db: e.tensor_tensor(out=x[:, db, :], in0=pd[:], in1=x[:, db, :], op=ALU.add), [pd, x], [x])
                if db % 2 == 1:
                    yield

    def ple(self, x, s, c):
        p = self.p
        t0 = c * T
        hb = self.hb
        self.rmsnorm(x, PV["ple"], hb)
        pst = p.sb("pst", [128, 2, T])
        pb = p.sb("pb", [128, 2, T], BF16)
        p.dma("sync", pst[:], self.pT[s, :, t0:t0 + T].rearrange("(k p) t -> p k t", p=128), writes=[pst])
        self.G(lambda e: e.tensor_copy(out=pb[:], in_=pst[:]), [pst], [pb])
        sgp = [p.sb("sgp%d" % i, [128, T]) for i in range(2)]
        tp = [p.sb("tp%d" % i, [128, T]) for i in range(2)]
        for sl in range(4):
            wpb, wpv = self.load_w(self.plp[:, sl * 256:(sl + 1) * 256], 2, 256, ("plp", sl))
            wgb, wgv = self.load_w(self.plg[:, sl * 256:(sl + 1) * 256], 8, 256, ("plg", sl))
            for j in range(2):
                ob = sl * 2 + j
                pg = self.next_ps()
                for k in range(8):
                    self.MM(pg[:], wgv[:, k, j * 128:(j + 1) * 128], hb[:, k, :], k == 0, k == 7, [wgb, hb], [pg])
                pp = self.next_ps()
                for k in range(2):
                    self.MM(pp[:], wpv[:, k, j * 128:(j + 1) * 128], pb[:, k, :], k == 0, k == 1, [wpb, pb], [pp])
                sgb, tpb = sgp[ob % 2], tp[ob % 2]
                self.act(sgb, sgb[:], pg, pg[:], AF.Sigmoid)
                self.V(lambda e, pp=pp, sgb=sgb, tpb=tpb: e.tensor_tensor(out=tpb[:], in0=pp[:], in1=sgb[:], op=ALU.mult), [pp, sgb], [tpb])
                self.G(lambda e, tpb=tpb, ob=ob: e.tensor_tensor(out=x[:, ob, :], in0=x[:, ob, :], in1=tpb[:], op=ALU.add), [tpb, x], [x])

    def final(self, x, s, c, is_last):
        p = self.p
        t0 = c * T
        of = p.sb("of", [128, 8, T])
        self.rmsnorm(x, PV["fin"], None, out_f=of)
        if is_last:
            self.debug_out("x_fin", x, x[:], [128, 8, T])
            self.debug_out("rt_fin", self.rt, self.rt[:], [128, T])
            self.debug_out("of_fin", of, of[:], [128, 8, T])
            self.debug_out("pvec", self.pvec, self.pvec[:], [128, NPV])
        p.dma("sync", self.outT[s, :, t0:t0 + T].rearrange("(k p) t -> p k t", p=128), of[:], reads=[of], writes=[self.out_b])

    def setup_rwkv(self, stage):
        raise NotImplementedError

    def setup_s5(self, stage):
        raise NotImplementedError

    def mixer(self, s, c):
        raise NotImplementedError


TS = 128


class FullBuilder(Builder):
    def make_views(self):
        p = self.p
        self.q = []
        self.h = []
        for i, b in enumerate(self.banks):
            qs = [Buf("q%d_%d" % (i, j), b[:, j * 128:(j + 1) * 128]) for j in range(4)]
            hs = [Buf("h%d_%d" % (i, j), b[:, j * 256:(j + 1) * 256]) for j in range(2)]
            for v in qs + hs:
                v.root = b
            self.q.append(qs)
            self.h.append(hs)

    def setup_rwkv(self, stage):
        p = self.p
        if stage == 0:
            if not hasattr(self, "q"):
                self.make_views()
            self.lw_b = p.sb("lw_b", [128, 512], BF16)
            self.la_b = p.sb("la_b", [128, 512], BF16)
            self.lg_b = p.sb("lg_b", [128, 512], BF16)
            self.hmask = p.sb("hmask", [128, 2])
            self.mask4 = p.sb("mask4", [128, 512], BF16)
            self.m_sl = p.sb("m_sl", [128, 128], BF16)
            self.bd_b = p.sb("bd_b", [128, 128], BF16)
            self.bd64 = p.sb("bd64", [128, 128])
            self.rmask = p.sb("rmask", [128, T], BF16)
            self.onemka = p.sb("onemka", [128, 4])
            self.carrys = [p.sb("carry%d" % s, [128, 14]) for s in range(self.nseq)]
            self.H2fs = [[p.sb("H2f%d_%d" % (s, i), [128, 128]) for i in range(4)] for s in range(self.nseq)]
            self.H2bs = [[p.sb("H2b%d_%d" % (s, i), [128, 128], BF16) for i in range(4)] for s in range(self.nseq)]
            self.carry, self.H2f, self.H2b = self.carrys[0], self.H2fs[0], self.H2bs[0]
            return
        p = self.p
        if not hasattr(self, "q"):
            self.make_views()
        st = self.wst[0]
        p.dma("sync", st[:, 0:512], self.lora_wa, writes=[st])
        self.G(lambda e: e.memset(self.lw_b[:], 0.0), [], [self.lw_b])
        self.G(lambda e: e.memset(self.la_b[:], 0.0), [], [self.la_b])
        self.G(lambda e: e.tensor_copy(out=self.lw_b[0:64, :], in_=st[0:64, 0:512]), [st, self.lw_b], [self.lw_b])
        self.G(lambda e: e.tensor_copy(out=self.la_b[64:128, :], in_=st[64:128, 0:512]), [st, self.la_b], [self.la_b])
        self.G(lambda e: e.memset(self.hmask[:], 0.0), [], [self.hmask])
        self.G(lambda e: e.memset(self.hmask[0:64, 0:1], 1.0), [self.hmask], [self.hmask])
        self.G(lambda e: e.memset(self.hmask[64:128, 1:2], 1.0), [self.hmask], [self.hmask])
        st1 = self.wst[1]
        p.dma("sync", st1[:, 0:512], self.lora_g, writes=[st1])
        self.G(lambda e: e.tensor_copy(out=self.lg_b[:], in_=st1[:, 0:512]), [st1], [self.lg_b])
        mf = p.sb("mf", [128, 128])
        for (cmp_, dsts) in [(ALU.is_gt, [0, 2]), (ALU.is_ge, [1, 3])]:
            self.G(lambda e: e.memset(mf[:], 1.0), [], [mf])
            self.G(lambda e, cmp_=cmp_: e.affine_select(out=mf[:], in_=mf[:], pattern=[[1, 128]], compare_op=cmp_, fill=0.0,
                                                        base=0, channel_multiplier=-1), [mf], [mf])
            self.G(lambda e: e.memset(mf[0:64, 64:128], 0.0), [mf], [mf])
            for d_ in dsts:
                self.G(lambda e, d_=d_: e.tensor_copy(out=self.mask4[:, d_ * 128:(d_ + 1) * 128], in_=mf[:]), [mf], [self.mask4])
        self.G(lambda e: e.memset(mf[:], 1.0), [], [mf])
        self.G(lambda e: e.affine_select(out=mf[:], in_=mf[:], pattern=[[-1, 128]], compare_op=ALU.is_gt, fill=0.0,
                                         base=0, channel_multiplier=1), [mf], [mf])
        self.G(lambda e: e.memset(mf[64:128, 0:64], 0.0), [mf], [mf])
        self.G(lambda e: e.tensor_copy(out=self.m_sl[:], in_=mf[:]), [mf], [self.m_sl])
        self.G(lambda e: e.memset(mf[:], 0.0), [mf], [mf])
        self.G(lambda e: e.memset(mf[0:64, 0:64], 1.0), [mf], [mf])
        self.G(lambda e: e.memset(mf[64:128, 64:128], 1.0), [mf], [mf])
        self.G(lambda e: e.tensor_copy(out=self.bd_b[:], in_=mf[:]), [mf], [self.bd_b])
        self.G(lambda e: e.tensor_scalar(out=self.bd64[:], in0=mf[:], scalar1=1.0 / 64, scalar2=None, op0=ALU.mult), [mf], [self.bd64])
        self.G(lambda e: e.memset(self.rmask[:], 1.0), [], [self.rmask])
        self.G(lambda e: e.memset(self.rmask[:].rearrange("p (c l) -> p c l", l=64)[:, :, 0:1], 0.0), [self.rmask], [self.rmask])
        pv = self.pvec
        self.V(lambda e: e.tensor_scalar(out=self.onemka[:], in0=pv[:, PV["ka"]:PV["ka"] + 4], scalar1=-1.0, scalar2=1.0,
                                         op0=ALU.mult, op1=ALU.add), [pv], [self.onemka])


    def setup_s5(self, stage):
        p = self.p
        if stage == 0:
            if not hasattr(self, "q"):
                self.make_views()
            self.cosT = p.sb("cosT", [128, 8, TS])
            self.sinT = p.sb("sinT", [128, 8, TS])
            self.rho = p.sb("rho", [128, 8])
            self.cTs = p.sb("cTs", [128, 8])
            self.sTs = p.sb("sTs", [128, 8])
            self.nsTs = p.sb("nsTs", [128, 8])
            self.cwre = p.sb("cwre", [128, 8, 128], BF16)
            self.cwimn = p.sb("cwimn", [128, 8, 128], BF16)
            self.bre_b = p.sb("bre_b", [128, 2, 512], BF16)
            self.bim_b = p.sb("bim_b", [128, 2, 512], BF16)
            self.glu_b16 = p.sb("glu_b16", [128, 2, 256], BF16)
            self.s5c_re = p.sb("s5c_re", [128, 8])
            self.s5c_im = p.sb("s5c_im", [128, 8])
            self.s5cars = [p.sb("s5car%d" % s, [128, 2, 8]) for s in range(self.nseq)]
            self.s5car = self.s5cars[0]
            return
        p = self.p
        if not hasattr(self, "q"):
            self.make_views()
        pv = self.pvec
        for (dr, dst) in [(self.s5bre, self.bre_b), (self.s5bim, self.bim_b)]:
            i = self._srr % 2
            self._srr += 1
            st = self.wst[i]
            p.dma("sync", st[:, 0:1024].rearrange("p (k c) -> p k c", k=2), dr.rearrange("(k p) c -> p k c", p=128), writes=[st])
            self.G(lambda e, st=st, dst=dst: e.tensor_copy(out=dst[:].rearrange("p k c -> p (k c)"), in_=st[:, 0:1024]), [st], [dst])
        i = self._srr % 2
        self._srr += 1
        st = self.wst[i]
        p.dma("sync", st[:, 0:512].rearrange("p (k c) -> p k c", k=2), self.glu_w.rearrange("(k p) c -> p k c", p=128), writes=[st])
        self.G(lambda e, st=st: e.tensor_copy(out=self.glu_b16[:].rearrange("p k c -> p (k c)"), in_=st[:, 0:512]), [st], [self.glu_b16])
        with p.scope():
            cre = p.sb("cre", [128, 8, 128])
            cim = p.sb("cim", [128, 8, 128])
            tt = p.sb("s5tt", [128, 16, 8])
            big1 = p.sb("big1", [128, 8, 128])
            big2 = p.sb("big2", [128, 8, 128])
            st0, st1 = self.wst[0], self.wst[1]
            self._srr += 2
            p.dma("sync", st0[:, 0:1024], self.s5cre, writes=[st0])
            p.dma("sync", st1[:, 0:1024], self.s5cim, writes=[st1])
            self.V(lambda e: e.tensor_copy(out=cre[:].rearrange("p a b -> p (a b)"), in_=st0[:, 0:1024]), [st0], [cre])
            self.V(lambda e: e.tensor_copy(out=cim[:].rearrange("p a b -> p (a b)"), in_=st1[:, 0:1024]), [st1], [cim])
            lre = pv[:, PV["lre"]:PV["lre"] + 8]
            lim = pv[:, PV["lim"]:PV["lim"] + 8]
            ldt = pv[:, PV["ldt"]:PV["ldt"] + 8]
            t = lambda i: tt[:, i, :]
            TT = lambda fn: self.V(fn, [tt, pv, self.eps_col], [tt])
            self.A(lambda e: e.activation(out=t(0), in_=ldt, func=AF.Exp), [pv], [tt])
            TT(lambda e: e.tensor_tensor(out=t(1), in0=lre, in1=t(0), op=ALU.mult))
            TT(lambda e: e.tensor_tensor(out=t(2), in0=lim, in1=t(0), op=ALU.mult))
            self.A(lambda e: e.activation(out=self.rho[:], in_=t(1), func=AF.Exp), [tt], [self.rho])
            self.A(lambda e: e.activation(out=t(3), in_=t(2), func=AF.Sin, scale=1.0 / 16), [tt], [tt])
            self.A(lambda e: e.activation(out=t(4), in_=t(2), func=AF.Sin, scale=-1.0 / 16, bias=self.eps_col[:, 2:3]), [tt, self.eps_col], [tt])

            def square(ci, si):
                TT(lambda e: e.tensor_tensor(out=t(5), in0=t(ci), in1=t(ci), op=ALU.mult))
                TT(lambda e: e.tensor_tensor(out=t(6), in0=t(si), in1=t(si), op=ALU.mult))
                TT(lambda e: e.tensor_tensor(out=t(7), in0=t(ci), in1=t(si), op=ALU.mult))
                TT(lambda e: e.tensor_tensor(out=t(ci), in0=t(5), in1=t(6), op=ALU.subtract))
                TT(lambda e: e.tensor_scalar(out=t(si), in0=t(7), scalar1=2.0, scalar2=None, op0=ALU.mult))
            for _ in range(4):
                square(4, 3)
            TT(lambda e: e.tensor_tensor(out=t(8), in0=self.rho[:], in1=t(4), op=ALU.mult))
            TT(lambda e: e.tensor_tensor(out=t(9), in0=self.rho[:], in1=t(3), op=ALU.mult))
            self.V(lambda e: e.tensor_scalar(out=t(8), in0=t(8), scalar1=-1.0, scalar2=None, op0=ALU.add), [tt, self.rho], [tt])
            TT(lambda e: e.tensor_tensor(out=t(10), in0=lre, in1=lre, op=ALU.mult))
            TT(lambda e: e.tensor_tensor(out=t(11), in0=lim, in1=lim, op=ALU.mult))
            TT(lambda e: e.tensor_tensor(out=t(10), in0=t(10), in1=t(11), op=ALU.add))
            TT(lambda e: e.reciprocal(out=t(10), in_=t(10)))
            TT(lambda e: e.tensor_tensor(out=t(11), in0=t(8), in1=lre, op=ALU.mult))
            TT(lambda e: e.tensor_tensor(out=t(12), in0=t(9), in1=lim, op=ALU.mult))
            TT(lambda e: e.tensor_tensor(out=t(11), in0=t(11), in1=t(12), op=ALU.add))
            self.V(lambda e: e.tensor_tensor(out=self.s5c_re[:], in0=t(11), in1=t(10), op=ALU.mult), [tt], [self.s5c_re])
            TT(lambda e: e.tensor_tensor(out=t(11), in0=t(9), in1=lre, op=ALU.mult))
            TT(lambda e: e.tensor_tensor(out=t(12), in0=t(8), in1=lim, op=ALU.mult))
            TT(lambda e: e.tensor_tensor(out=t(11), in0=t(11), in1=t(12), op=ALU.subtract))
            self.V(lambda e: e.tensor_tensor(out=self.s5c_im[:], in0=t(11), in1=t(10), op=ALU.mult), [tt], [self.s5c_im])
            cr_b = self.s5c_re[:].unsqueeze(2).to_broadcast([128, 8, 128])
            ci_b = self.s5c_im[:].unsqueeze(2).to_broadcast([128, 8, 128])
            self.V(lambda e: e.tensor_tensor(out=big1[:], in0=cre[:], in1=cr_b, op=ALU.mult), [cre, self.s5c_re], [big1])
            self.V(lambda e: e.tensor_tensor(out=big2[:], in0=cim[:], in1=ci_b, op=ALU.mult), [cim, self.s5c_im], [big2])
            self.V(lambda e: e.tensor_tensor(out=self.cwre[:], in0=big1[:], in1=big2[:], op=ALU.subtract), [big1, big2], [self.cwre])
            self.V(lambda e: e.tensor_tensor(out=big1[:], in0=cre[:], in1=ci_b, op=ALU.mult), [cre, self.s5c_im], [big1])
            self.V(lambda e: e.tensor_tensor(out=big2[:], in0=cim[:], in1=cr_b, op=ALU.mult), [cim, self.s5c_re], [big2])
            self.V(lambda e: e.scalar_tensor_tensor(out=self.cwimn[:], in0=big1[:], scalar=-1.0, in1=big2[:], op0=ALU.mult, op1=ALU.subtract),
                   [big1, big2], [self.cwimn])
            cosT, sinT = self.cosT, self.sinT
            self.V(lambda e: e.memset(cosT[:, :, 0:1], 1.0), [], [cosT])
            self.V(lambda e: e.memset(sinT[:, :, 0:1], 0.0), [], [sinT])
            L = 1
            tmpa = p.sb("tmpa", [128, 8, 128])
            tmpb = p.sb("tmpb", [128, 8, 128])
            while L < TS:
                pc = t(4).unsqueeze(2).to_broadcast([128, 8, L])
                ps_ = t(3).unsqueeze(2).to_broadcast([128, 8, L])
                self.V(lambda e, L=L, pc=pc: e.tensor_tensor(out=tmpa[:, :, 0:L], in0=cosT[:, :, 0:L], in1=pc, op=ALU.mult), [cosT, tt], [tmpa])
                self.V(lambda e, L=L, ps_=ps_: e.tensor_tensor(out=tmpb[:, :, 0:L], in0=sinT[:, :, 0:L], in1=ps_, op=ALU.mult), [sinT, tt], [tmpb])
                self.V(lambda e, L=L: e.tensor_tensor(out=cosT[:, :, L:2 * L], in0=tmpa[:, :, 0:L], in1=tmpb[:, :, 0:L], op=ALU.subtract), [tmpa, tmpb, cosT], [cosT])
                self.V(lambda e, L=L, ps_=ps_: e.tensor_tensor(out=tmpa[:, :, 0:L], in0=cosT[:, :, 0:L], in1=ps_, op=ALU.mult), [cosT, tt], [tmpa])
                self.V(lambda e, L=L, pc=pc: e.tensor_tensor(out=tmpb[:, :, 0:L], in0=sinT[:, :, 0:L], in1=pc, op=ALU.mult), [sinT, tt], [tmpb])
                self.V(lambda e, L=L: e.tensor_tensor(out=sinT[:, :, L:2 * L], in0=tmpa[:, :, 0:L], in1=tmpb[:, :, 0:L], op=ALU.add), [tmpa, tmpb, sinT], [sinT])
                square(4, 3)
                L *= 2
            self.V(lambda e: e.tensor_copy(out=self.cTs[:], in_=t(4)), [tt], [self.cTs])
            self.V(lambda e: e.tensor_copy(out=self.sTs[:], in_=t(3)), [tt], [self.sTs])
            self.V(lambda e: e.tensor_scalar(out=self.nsTs[:], in0=t(3), scalar1=-1.0, scalar2=None, op0=ALU.mult), [tt], [self.nsTs])
        self.debug_out("cosT", self.cosT, self.cosT[:], [128, 8, TS])
        self.debug_out("sinT", self.sinT, self.sinT[:], [128, 8, TS])
        self.debug_out("cwre", self.cwre, self.cwre[:], [128, 8, 128], BF16)


    def set_seq(self, s):
        if self.en_rwkv:
            self.carry, self.H2f, self.H2b = self.carrys[s], self.H2fs[s], self.H2bs[s]
        if self.en_s5:
            self.s5car = self.s5cars[s]

    def inproj_block(self, blk, dst_b, dst_ap, first_chunk):
        p = self.p
        hb, pv = self.hb, self.pvec
        wb, wv = self.load_w(self.w_in[:, blk * 128:(blk + 1) * 128], 8, 128, ("win", blk))
        ps = self.next_ps()
        for k in range(8):
            self.MM(ps[:], wv[:, k, :], hb[:, k, :], k == 0, k == 7, [wb, hb], [ps])
        raw = self.rawb[self._rawi % 2]
        tmp = self.shtmp[0]
        self._rawi += 1
        carry = self.carry
        self.act(raw, raw[:, 1:T + 1], ps, ps[:], AF.Copy)
        self.V(lambda e: e.tensor_copy(out=raw[:, 0:1], in_=carry[:, blk:blk + 1]), [carry, raw], [raw])
        self.V(lambda e: e.tensor_copy(out=carry[:, blk:blk + 1], in_=raw[:, T:T + 1]), [raw, carry], [carry])
        self.V(lambda e: e.tensor_tensor(out=tmp[:], in0=raw[:, 0:T], in1=raw[:, 1:T + 1], op=ALU.subtract), [raw], [tmp])
        self.V(lambda e: e.scalar_tensor_tensor(out=dst_ap, in0=tmp[:], scalar=pv[:, PV["mu"] + blk:PV["mu"] + blk + 1], in1=raw[:, 1:T + 1],
                                                op0=ALU.mult, op1=ALU.add), [tmp, pv, raw], [dst_b])
        if blk == 0:
            self.debug_out("ip_raw", raw, raw[:], [128, T + 1])
            self.debug_out("ip_tmp", tmp, tmp[:], [128, T])
            self.debug_out("ip_dst", dst_b, dst_ap, [128, T])
            self.debug_out("ip_w", wb, wb[:, 0:1024], [128, 1024], BF16)
            self.debug_out("ip_hb", hb, hb[:], [128, 8, T], BF16)

    def mixer(self, s, c):
        p = self.p
        x, hb, pv = self.x, self.hb, self.pvec
        last = (s == self.nseq - 1 and c == self.seq // T - 1)
        self.rmsnorm(x, PV["mix"], hb)
        self.rwo = p.sb("rwo", [128, 4, T], BF16)
        self.s5o = p.sb("s5o", [128, 2, T], BF16)
        self.rawb = [p.sb("raw%d" % i, [128, T + 1]) for i in range(2)]
        self.shtmp = [p.sb("shtmp%d" % i, [128, T]) for i in range(1)]
        self._rawi = 0
        if self.en_rwkv:
            if c == 0:
                carry_, H2f_, H2b_ = self.carry, self.H2f, self.H2b
                self.G(lambda e: e.memset(carry_[:], 0.0), [carry_], [carry_])
                for hp in range(4):
                    self.G(lambda e, hp=hp: e.memset(H2f_[hp][:], 0.0), [H2f_[hp]], [H2f_[hp]])
                    self.G(lambda e, hp=hp: e.memset(H2b_[hp][:], 0.0), [H2b_[hp]], [H2b_[hp]])
            if getattr(self, "rw_stage", 9) < 9:
                rwo__ = self.rwo
                self.G(lambda e: e.memset(rwo__[:], 0.0), [], [rwo__])
            self.twd = p.sb("twd", [128, T], BF16)
            self.sgd = p.sb("sgd", [128, T], BF16)
            with p.scope():
                lo12 = p.sb("lo12", [128, T])
                lo13 = p.sb("lo13", [128, T])
                self.inproj_block(12, lo12, lo12[:], c == 0)
                self.inproj_block(13, lo13, lo13[:], c == 0)
                twd_, sgd_ = self.twd, self.sgd
                self.A(lambda e: e.activation(out=twd_[0:64, :], in_=lo12[0:64, :], func=AF.Tanh), [lo12], [twd_])
                self.A(lambda e: e.activation(out=twd_[64:128, :], in_=lo12[64:128, :], func=AF.Copy), [lo12], [twd_])
                self.A(lambda e: e.activation(out=sgd_[:], in_=lo13[:], func=AF.Sigmoid), [lo13], [sgd_])
            p.tick_every = self.tick_rwkv
            p.side_budget = 0
            for hp in range(4):
                p.side_budget += self.bud_rwkv
                with p.scope():
                    self.rwkv_hp(s, c, hp, last)
        else:
            rwo_ = self.rwo
            self.G(lambda e: e.memset(rwo_[:], 0.0), [], [rwo_])
        p.tick_every = self.tick_s5
        p.side_budget += self.bud_s5
        if self.en_s5:
            with p.scope():
                self.s5(s, c, last)
        else:
            s5o_ = self.s5o
            self.G(lambda e: e.memset(s5o_[:], 0.0), [], [s5o_])
        p.tick_every = self.tick_merge
        p.side_budget += 1000
        with p.scope():
            self.merge(s, c, last)

    def s5(self, s, c, last):
        p = self.p
        hb, pv = self.hb, self.pvec
        uf = p.sb("uf", [128, 2, T])
        ub = p.sb("ub", [128, 2, T], BF16)
        zb = p.sb("zb", [128, 2, T], BF16)
        wb, wv = self.load_w(self.w_in[:, 1792:2048], 8, 256, ("win_s5", 0))
        for j in range(2):
            ps = self.next_ps()
            for k in range(8):
                self.MM(ps[:], wv[:, k, j * 128:(j + 1) * 128], hb[:, k, :], k == 0, k == 7, [wb, hb], [ps])
            self.act(uf, uf[:, j, :], ps, ps[:], AF.Copy)
            self.G(lambda e, j=j: e.tensor_copy(out=ub[:, j, :], in_=uf[:, j, :]), [uf], [ub])
        car = self.s5car
        if c == 0:
            self.G(lambda e: e.memset(car[:], 0.0), [car], [car])
        tn = ["t1", "t2", "t3", "t4", "wre", "wim", "zre", "zim", "u1", "u2"]
        tb_ = {n: [p.sb(n + "_%d" % i, [128, TS]) for i in range(2)] for n in tn}
        xre = [p.sb("xre%d" % i, [128, TS], BF16) for i in range(2)]
        xim = [p.sb("xim%d" % i, [128, TS], BF16) for i in range(2)]
        sm = p.sb("s5sm", [128, 8])
        ysb = p.sb("ysb", [128, TS])
        it = 0
        for sc in range(T // TS):
            off = sc * TS
            for gp in range(8):
                kt, gpl = gp // 4, gp % 4
                i = it % 2
                it += 1
                B = {n: tb_[n][i] for n in tn}
                bank = self.next_ps()
                hre, him = None, None
                bi = self.banks.index(bank)
                hre, him = self.h[bi][0], self.h[bi][1]
                self.MM(bank[:, 0:TS], self.bre_b[:, kt, gpl * 128:(gpl + 1) * 128], ub[:, kt, off:off + TS], True, True, [self.bre_b, ub], [bank])
                self.MM(bank[:, TS:2 * TS], self.bim_b[:, kt, gpl * 128:(gpl + 1) * 128], ub[:, kt, off:off + TS], True, True, [self.bim_b, ub], [bank])
                cg_, sg_ = self.cosT[:, gp, :], self.sinT[:, gp, :]
                hre, him = bank, bank
                self.V(lambda e, B=B, bank=bank, cg_=cg_: e.tensor_tensor(out=B["t1"][:], in0=bank[:, 0:TS], in1=cg_, op=ALU.mult), [bank, self.cosT], [B["t1"]])
                self.V(lambda e, B=B, bank=bank, sg_=sg_: e.tensor_tensor(out=B["t2"][:], in0=bank[:, TS:2 * TS], in1=sg_, op=ALU.mult), [bank, self.sinT], [B["t2"]])
                self.V(lambda e, B=B, bank=bank, cg_=cg_: e.tensor_tensor(out=B["t3"][:], in0=bank[:, TS:2 * TS], in1=cg_, op=ALU.mult), [bank, self.cosT], [B["t3"]])
                self.V(lambda e, B=B, bank=bank, sg_=sg_: e.tensor_tensor(out=B["t4"][:], in0=bank[:, 0:TS], in1=sg_, op=ALU.mult), [bank, self.sinT], [B["t4"]])
                self.V(lambda e, B=B: e.tensor_tensor(out=B["wre"][:], in0=B["t1"][:], in1=B["t2"][:], op=ALU.add), [B["t1"], B["t2"]], [B["wre"]])
                self.V(lambda e, B=B: e.tensor_tensor(out=B["wim"][:], in0=B["t3"][:], in1=B["t4"][:], op=ALU.subtract), [B["t3"], B["t4"]], [B["wim"]])
                rb = self.rho[:, gp:gp + 1].to_broadcast([128, TS])
                self.V(lambda e, B=B, rb=rb, gp=gp: e.tensor_tensor_scan(out=B["zre"][:], data0=rb, data1=B["wre"][:], initial=car[:, 0, gp:gp + 1],
                                                                        op0=ALU.mult, op1=ALU.add), [self.rho, B["wre"], car], [B["zre"]])
                self.V(lambda e, B=B, rb=rb, gp=gp: e.tensor_tensor_scan(out=B["zim"][:], data0=rb, data1=B["wim"][:], initial=car[:, 1, gp:gp + 1],
                                                                        op0=ALU.mult, op1=ALU.add), [self.rho, B["wim"], car], [B["zim"]])
                zr, zi = B["zre"], B["zim"]
                cT, sT, nsT = self.cTs[:, gp:gp + 1], self.sTs[:, gp:gp + 1], self.nsTs[:, gp:gp + 1]
                self.V(lambda e, zr=zr, cT=cT: e.tensor_scalar(out=sm[:, 0:1], in0=zr[:, TS - 1:TS], scalar1=cT, scalar2=None, op0=ALU.mult), [zr, self.cTs, sm], [sm])
                self.V(lambda e, zi=zi, cT=cT: e.tensor_scalar(out=sm[:, 1:2], in0=zi[:, TS - 1:TS], scalar1=cT, scalar2=None, op0=ALU.mult), [zi, self.cTs, sm], [sm])
                self.V(lambda e, zi=zi, nsT=nsT, gp=gp: e.scalar_tensor_tensor(out=car[:, 0, gp:gp + 1], in0=zi[:, TS - 1:TS], scalar=nsT, in1=sm[:, 0:1], op0=ALU.mult, op1=ALU.add),
                       [zi, self.nsTs, sm, car], [car])
                self.V(lambda e, zr=zr, sT=sT, gp=gp: e.scalar_tensor_tensor(out=car[:, 1, gp:gp + 1], in0=zr[:, TS - 1:TS], scalar=sT, in1=sm[:, 1:2], op0=ALU.mult, op1=ALU.add),
                       [zr, self.sTs, sm, car], [car])
                xr, xi = xre[i], xim[i]
                self.G(lambda e, B=B, cg_=cg_: e.tensor_tensor(out=B["u1"][:], in0=B["zre"][:], in1=cg_, op=ALU.mult), [B["zre"], self.cosT], [B["u1"]])
                self.G(lambda e, B=B, sg_=sg_: e.tensor_tensor(out=B["u2"][:], in0=B["zim"][:], in1=sg_, op=ALU.mult), [B["zim"], self.sinT], [B["u2"]])
                self.G(lambda e, B=B, xr=xr: e.tensor_tensor(out=xr[:], in0=B["u1"][:], in1=B["u2"][:], op=ALU.subtract), [B["u1"], B["u2"]], [xr])
                self.V(lambda e, B=B, sg_=sg_: e.tensor_tensor(out=B["t1"][:], in0=B["zre"][:], in1=sg_, op=ALU.mult), [B["zre"], self.sinT], [B["t1"]])
                self.V(lambda e, B=B, cg_=cg_: e.tensor_tensor(out=B["t2"][:], in0=B["zim"][:], in1=cg_, op=ALU.mult), [B["zim"], self.cosT], [B["t2"]])
                self.V(lambda e, B=B, xi=xi: e.tensor_tensor(out=xi[:], in0=B["t1"][:], in1=B["t2"][:], op=ALU.add), [B["t1"], B["t2"]], [xi])
                if last and sc == 1 and gp == 0:
                    self.debug_out("s5_zre", B["zre"], B["zre"][:], [128, TS])
                    self.debug_out("s5_xre", xr, xr[:], [128, TS], BF16)
                pa = self.psa[kt]
                self.MM(pa[:, 0:TS], self.cwre[:, gp, :], xr[:], gpl == 0, False, [self.cwre, xr], [pa])
                self.MM(pa[:, 0:TS], self.cwimn[:, gp, :], xi[:], False, gpl == 3, [self.cwimn, xi], [pa])
                if gpl == 3:
                    self.V(lambda e, kt=kt, pa=pa, off=off: e.scalar_tensor_tensor(out=ysb[:], in0=uf[:, kt, off:off + TS], scalar=pv[:, PV["s5d"] + kt:PV["s5d"] + kt + 1],
                                                                                  in1=pa[:, 0:TS], op0=ALU.mult, op1=ALU.add), [uf, pv, pa], [ysb])
                    if last and sc == 1 and kt == 0:
                        self.debug_out("s5_y", ysb, ysb[:], [128, TS])
                    self.A(lambda e, kt=kt, off=off: e.activation(out=zb[:, kt, off:off + TS], in_=ysb[:], func=AF.Gelu), [ysb], [zb])
        sgl = p.sb("sgl", [128, T])
        for ob in range(2):
            ps = self.next_ps()
            for k in range(2):
                self.MM(ps[:], self.glu_b16[:, k, ob * 128:(ob + 1) * 128], zb[:, k, :], k == 0, k == 1, [self.glu_b16, zb], [ps])
            self.A(lambda e, ps=ps, ob=ob: e.activation(out=sgl[:], in_=ps[:], func=AF.Sigmoid, bias=pv[:, PV["glub"] + ob:PV["glub"] + ob + 1]), [ps, pv], [sgl])
            s5o_ = self.s5o
            self.V(lambda e, ob=ob: e.tensor_tensor(out=s5o_[:, ob, :], in0=zb[:, ob, :], in1=sgl[:], op=ALU.mult), [zb, sgl], [s5o_])
        if last:
            self.debug_out("s5o", self.s5o, self.s5o[:], [128, 2, T], BF16)

    def merge(self, s, c, last):
        p = self.p
        hb, x = self.hb, self.x
        mg = p.sb("mg", [128, 8, T], BF16)
        ga = [p.sb("ga%d" % i, [128, T], BF16) for i in range(2)]
        gb = [p.sb("gb%d" % i, [128, T], BF16) for i in range(2)]
        t1 = [p.sb("mt1_%d" % i, [128, T]) for i in range(2)]
        t2 = [p.sb("mt2_%d" % i, [128, T]) for i in range(2)]
        for ob in range(8):
            i = ob % 2
            wab, wav = self.load_w(self.w_in[:, 2048 + ob * 128:2048 + (ob + 1) * 128], 8, 128, ("win_ga", ob))
            pg = self.next_ps()
            for k in range(8):
                self.MM(pg[:], wav[:, k, :], hb[:, k, :], k == 0, k == 7, [wab, hb], [pg])
            self.act(ga[i], ga[i][:], pg, pg[:], AF.Sigmoid)
            wbb_, wbv = self.load_w(self.w_in[:, 3072 + ob * 128:3072 + (ob + 1) * 128], 8, 128, ("win_gb", ob))
            pg2 = self.next_ps()
            for k in range(8):
                self.MM(pg2[:], wbv[:, k, :], hb[:, k, :], k == 0, k == 7, [wbb_, hb], [pg2])
            self.act(gb[i], gb[i][:], pg2, pg2[:], AF.Sigmoid)
            w3b, w3v = self.load_w(self.w_ba[:, ob * 128:(ob + 1) * 128], 4, 128, ("wba", ob))
            pa = self.next_ps()
            for k in range(4):
                self.MM(pa[:], w3v[:, k, :], self.rwo[:, k, :], k == 0, k == 3, [w3b, self.rwo], [pa])
            self.V(lambda e, pa=pa, i=i: e.tensor_tensor(out=t1[i][:], in0=pa[:], in1=ga[i][:], op=ALU.mult), [pa, ga[i]], [t1[i]])
            w4b, w4v = self.load_w(self.w_bb[:, ob * 128:(ob + 1) * 128], 2, 128, ("wbb", ob))
            pb_ = self.next_ps()
            for k in range(2):
                self.MM(pb_[:], w4v[:, k, :], self.s5o[:, k, :], k == 0, k == 1, [w4b, self.s5o], [pb_])
            self.V(lambda e, pb_=pb_, i=i: e.tensor_tensor(out=t2[i][:], in0=pb_[:], in1=gb[i][:], op=ALU.mult), [pb_, gb[i]], [t2[i]])
            self.G(lambda e, i=i, ob=ob: e.tensor_tensor(out=mg[:, ob, :], in0=t1[i][:], in1=t2[i][:], op=ALU.add), [t1[i], t2[i]], [mg])
        if last:
            self.debug_out("mg", mg, mg[:], [128, 8, T], BF16)
            self.debug_out("rwo", self.rwo, self.rwo[:], [128, 4, T], BF16)
        for sl in range(4):
            wob, wov = self.load_w(self.w_out[:, sl * 256:(sl + 1) * 256], 8, 256, ("wout", sl))
            for j in range(2):
                ob = sl * 2 + j
                ps = self.next_ps()
                for k in range(8):
                    self.MM(ps[:], wov[:, k, j * 128:(j + 1) * 128], mg[:, k, :], k == 0, k == 7, [wob, mg], [ps])
                self.V(lambda e, ps=ps, ob=ob: e.tensor_tensor(out=x[:, ob, :], in0=ps[:], in1=x[:, ob, :], op=ALU.add), [ps, x], [x])

    def rwkv_hp(self, s, c, hp, last):
        p = self.p
        pv, hb = self.pvec, self.hb
        twd, sgd, rwo = self.twd, self.sgd, self.rwo
        H2f, H2b = self.H2f[hp], self.H2b[hp]
        hmask = self.hmask
        N = lambda nm, dt=F32, shape=(128, T): p.sb(nm, list(shape), dt)
        arT = N("arT", BF16, (128, 2, T))
        bT = N("bT", BF16)
        pad = {(q, e): N("pad%s%d" % (q, e), BF16) for q in "abk" for e in range(2)}
        tokV = N("tokV", BF16, (128, 4, 128))
        tokA = N("tokA", BF16, (128, 4, 128))
        apT = N("apT", BF16, (128, 4, 128))
        U0 = N("U0", BF16, (128, 4, 128))
        tokB = [N("tokB%d" % j, BF16, (128, 4, 128)) for j in range(2)]
        tokK = [N("tokK%d" % j, BF16, (128, 4, 128)) for j in range(2)]
        A4 = [N("A4_%d" % u, BF16, (128, 512)) for u in range(8)]
        TTm = [N("TT_%d" % u, BF16, (128, 128)) for u in range(8)]
        wl = N("wl", F32, (128, 8))
        g_sb = N("g_sb")
        bonus = N("bonus")
        y_sb = N("y_sb")
        U_sb = N("U_sb", BF16, (128, 128))
        self.G(lambda e: e.memset(U_sb[:], 0.0), [], [U_sb])
        with p.scope():
            rS, kS, vS = N("rS"), N("kS"), N("vS")
            self.inproj_block(hp, rS, rS[:], c == 0)
            self.inproj_block(4 + hp, kS, kS[:], c == 0)
            self.inproj_block(8 + hp, vS, vS[:], c == 0)
            cs_ = slice(hp * 128, (hp + 1) * 128)
            ps_w, ps_a, ps_g = self.next_ps(), self.next_ps(), self.next_ps()
            self.MM(ps_w[:], self.lw_b[:, cs_], twd[:], True, True, [self.lw_b, twd], [ps_w])
            self.MM(ps_a[:], self.la_b[:, cs_], twd[:], True, True, [self.la_b, twd], [ps_a])
            self.MM(ps_g[:], self.lg_b[:, cs_], sgd[:], True, True, [self.lg_b, sgd], [ps_g])
            lws, asg, cum, kk, nrm, kkn = N("lws"), N("asg"), N("cum"), N("kk"), N("nrm"), N("kkn")
            kk2 = N("kk2", BF16)
            kmod = N("kmod")
            tmp = self.shtmp[0]
            ea = Buf("ea_v", self.rawb[0][:, 0:T])
            ea.root = self.rawb[0]
            ecp = Buf("ecp_v", self.rawb[1][:, 0:T])
            ecp.root = self.rawb[1]
            tka, cl, bvec, ecm, ecl = nrm, tmp, kk, lws, ea
            kT = N("kT", BF16)
            rk2, vb = kk2, kT
            bh = Buf("bh_v", kS[:, 0:T // 2].bitcast(BF16))
            bh.root = kS
            kh = Buf("kh_v", kS[:, T // 2:T].bitcast(BF16))
            kh.root = kS
            col = lambda nm: pv[:, PV[nm] + hp:PV[nm] + hp + 1]
            self.act(lws, lws[:], ps_w, ps_w[:], AF.Sigmoid, bias=col("w0"), extra_reads=[pv])
            self.act(asg, asg[:], ps_a, ps_a[:], AF.Sigmoid, bias=col("a0"), extra_reads=[pv])
            self.act(g_sb, g_sb[:], ps_g, ps_g[:], AF.Copy)
            self.V(lambda e: e.tensor_tensor_scan(out=cum[:], data0=self.rmask[:], data1=lws[:], initial=0.0, op0=ALU.mult, op1=ALU.add),
                   [self.rmask, lws], [cum])
            self.A(lambda e: e.activation(out=kk[:], in_=kS[:], func=AF.Copy, scale=col("kk")), [kS, pv], [kk])
            self.act(kk2, kk2[:], kk, kk[:], AF.Square)
            ps_ss = self.next_ps()
            self.MM(ps_ss[:], self.bd_b[:], kk2[:], True, True, [self.bd_b, kk2], [ps_ss])
            self.act(nrm, nrm[:], ps_ss, ps_ss[:], AF.Sqrt)
            self.V(lambda e: e.tensor_scalar(out=nrm[:], in0=nrm[:], scalar1=1e-12, scalar2=None, op0=ALU.max), [nrm], [nrm])
            self.V(lambda e: e.reciprocal(out=nrm[:], in_=nrm[:]), [nrm], [nrm])
            self.V(lambda e: e.tensor_tensor(out=kkn[:], in0=kk[:], in1=nrm[:], op=ALU.mult), [kk, nrm], [kkn])
            self.V(lambda e: e.tensor_scalar(out=tka[:], in0=asg[:], scalar1=col("ka"), scalar2=self.onemka[:, hp:hp + 1], op0=ALU.mult, op1=ALU.add),
                   [asg, pv, self.onemka], [tka])
            self.V(lambda e: e.tensor_tensor(out=kmod[:], in0=kS[:], in1=tka[:], op=ALU.mult), [kS, tka], [kmod])
            self.G(lambda e: e.tensor_tensor(out=bvec[:], in0=kkn[:], in1=asg[:], op=ALU.mult), [kkn, asg], [bvec])
            self.V(lambda e: e.tensor_tensor(out=tmp[:], in0=cum[:], in1=lws[:], op=ALU.subtract), [cum, lws], [tmp])
            self.act(ea, ea[:], tmp, tmp[:], AF.Exp, scale=-CDEC)
            self.V(lambda e: e.scalar_tensor_tensor(out=arT[:, 0, :], in0=kkn[:], scalar=-1.0, in1=ea[:], op0=ALU.mult, op1=ALU.mult), [kkn, ea], [arT])
            self.act(ecp, ecp[:], cum, cum[:], AF.Exp, scale=-CDEC)
            self.V(lambda e: e.tensor_tensor(out=arT[:, 1, :], in0=rS[:], in1=ecp[:], op=ALU.mult), [rS, ecp], [arT])
            self.G(lambda e: e.tensor_copy(out=wl[:].unsqueeze(2), in_=ecp[:].rearrange("p (c l) -> p c l", l=64)[:, :, 63:64]), [ecp], [wl])
            self.act(ecm, ecm[:], cum, cum[:], AF.Exp, scale=CDEC)
            self.V(lambda e: e.tensor_tensor(out=bT[:], in0=bvec[:], in1=ecm[:], op=ALU.mult), [bvec, ecm], [bT])
            self.G(lambda e: e.tensor_tensor(out=kT[:], in0=kmod[:], in1=ecm[:], op=ALU.mult), [kmod, ecm], [kT])
            cv = cum[:].rearrange("p (c l) -> p c l", l=64)
            self.V(lambda e: e.tensor_tensor(out=cl[:].rearrange("p (c l) -> p c l", l=64), in0=cv[:, :, 63:64].to_broadcast([128, 8, 64]), in1=cv, op=ALU.subtract),
                   [cum], [cl])
            self.act(ecl, ecl[:], cl, cl[:], AF.Exp, scale=-CDEC)
            self.V(lambda e: e.tensor_tensor(out=bh[:], in0=bvec[:], in1=ecl[:], op=ALU.mult), [bvec, ecl], [bh])
            self.V(lambda e: e.tensor_tensor(out=kh[:], in0=kmod[:], in1=ecl[:], op=ALU.mult), [kmod, ecl], [kh])
            for e_ in range(2):
                hm = hmask[:, e_:e_ + 1]
                self.A(lambda e, e_=e_, hm=hm: e.activation(out=pad[("a", e_)][:], in_=arT[:, 0, :], func=AF.Copy, scale=hm), [arT, hmask], [pad[("a", e_)]])
                self.A(lambda e, e_=e_, hm=hm: e.activation(out=pad[("b", e_)][:], in_=bT[:], func=AF.Copy, scale=hm), [bT, hmask], [pad[("b", e_)]])
                self.A(lambda e, e_=e_, hm=hm: e.activation(out=pad[("k", e_)][:], in_=kT[:], func=AF.Copy, scale=hm), [kT, hmask], [pad[("k", e_)]])
            self.act(vb, vb[:], vS, vS[:], AF.Copy)
            self.G(lambda e: e.tensor_tensor(out=tmp[:], in0=rS[:], in1=kmod[:], op=ALU.mult), [rS, kmod], [tmp])
            self.A(lambda e: e.activation(out=rk2[:], in_=tmp[:], func=AF.Copy, scale=col("rk")), [tmp, pv], [rk2])
            ps_b = self.next_ps()
            self.MM(ps_b[:], self.bd_b[:], rk2[:], True, True, [self.bd_b, rk2], [ps_b])
            self.V(lambda e: e.tensor_tensor(out=bonus[:], in0=ps_b[:], in1=vS[:], op=ALU.mult), [ps_b, vS], [bonus])
            if getattr(self, "rw_stage", 9) < 1:
                return
            for qi, (src, dsts) in enumerate([(vb, [(tokV, None)]), (bh, [(tokB[0], 0), (tokB[1], 1)]), (kh, [(tokK[0], 0), (tokK[1], 1)]), (None, [(tokA, None)])]):
                bank = self.next_ps()
                tv = Buf("tv", bank[:, 0:256].bitcast(BF16))
                tv.root = bank
                for tb in range(4):
                    if src is None:
                        self.p.op("tensor", lambda e, tb=tb, tv=tv: e.transpose(out=tv[:, tb * 128:(tb + 1) * 128], in_=arT[:, 0, tb * 128:(tb + 1) * 128], identity=self.ident_b[:]),
                                  [arT, self.ident_b], [tv])
                    else:
                        self.p.op("tensor", lambda e, tb=tb, src=src, tv=tv: e.transpose(out=tv[:, tb * 128:(tb + 1) * 128], in_=src[:, tb * 128:(tb + 1) * 128], identity=self.ident_b[:]),
                                  [src, self.ident_b], [tv])
                for (dst, j) in dsts:
                    if j is None:
                        self.act(dst, dst[:].rearrange("p a b -> p (a b)"), tv, tv[:], AF.Copy)
                    else:
                        self.V(lambda e, dst=dst, j=j, tv=tv: e.tensor_scalar(out=dst[:].rearrange("p a b -> p (a b)"), in0=tv[:], scalar1=hmask[:, j:j + 1], scalar2=None, op0=ALU.mult),
                               [tv, hmask], [dst])
        if getattr(self, "rw_stage", 9) < 2:
            return
        with p.scope():
            PT = [N("PT%d" % u, BF16, (128, 128)) for u in range(8)]
            QQ = [[N("QQ%d_%d" % (u, i), BF16, (128, 256)) for i in range(2)] for u in range(8)]
            GG = [[N("GG%d_%d" % (u, i), BF16, (128, 128)) for i in range(2)] for u in range(8)]
            for rnd in range(2):
                for ui in range(4):
                    u = rnd * 4 + ui
                    tb, e_ = u // 2, u % 2
                    tbs = slice(tb * 128, (tb + 1) * 128)
                    bank = self.banks[ui]
                    self.MM(bank[:, 0:256], pad[("b", e_)][:, tbs], arT[:, :, tbs], True, True, [pad[("b", e_)], arT], [bank])
                    self.MM(bank[:, 256:512], pad[("k", e_)][:, tbs], arT[:, :, tbs], True, True, [pad[("k", e_)], arT], [bank])
                for ui in range(4):
                    u = rnd * 4 + ui
                    bank = self.banks[ui]
                    self.V(lambda e, u=u, bank=bank: e.tensor_tensor(out=A4[u][:], in0=bank[:], in1=self.mask4[:], op=ALU.mult), [bank, self.mask4], [A4[u]])
            for u in range(8):
                tb, e_ = u // 2, u % 2
                tbs = slice(tb * 128, (tb + 1) * 128)
                qv = self.q[4 + u // 4][u % 4]
                self.MM(qv[:], pad[("a", e_)][:, tbs], bT[:, tbs], True, True, [pad[("a", e_)], bT], [qv])
            for u in range(8):
                qv = self.q[4 + u // 4][u % 4]
                self.V(lambda e, u=u, qv=qv: e.tensor_tensor(out=PT[u][:], in0=qv[:], in1=self.m_sl[:], op=ALU.mult), [qv, self.m_sl], [PT[u]])
                self.G(lambda e, u=u: e.tensor_tensor(out=GG[u][0][:], in0=A4[u][:, 0:128], in1=self.ident_b[:], op=ALU.add), [A4[u], self.ident_b], [GG[u][0]])
            Qc = [(A4[u], A4[u][:, 0:128]) for u in range(8)]
            QTc = [(PT[u], PT[u][:]) for u in range(8)]
            for i in range(1, 6):
                for u in range(8):
                    hv = self.h[u // 2][u % 2]
                    if i < 5:
                        self.MM(hv[:, 0:128], QTc[u][1], Qc[u][1], True, True, [QTc[u][0], Qc[u][0]], [hv])
                    self.MM(hv[:, 128:256], Qc[u][1], QTc[u][1], True, True, [QTc[u][0], Qc[u][0]], [hv])
                for u in range(8):
                    hv = self.h[u // 2][u % 2]
                    qq = QQ[u][i % 2]
                    if i < 5:
                        self.act(qq, qq[:], hv, hv[:], AF.Copy)
                    else:
                        self.act(qq, qq[:, 128:256], hv, hv[:, 128:256], AF.Copy)
                    Qc[u] = (qq, qq[:, 0:128])
                    QTc[u] = (qq, qq[:, 128:256])
                for u in range(8):
                    qv = self.q[4 + u // 4][u % 4]
                    self.MM(qv[:], QTc[u][1], GG[u][(i - 1) % 2][:], True, True, [QTc[u][0], GG[u][(i - 1) % 2]], [qv])
                for u in range(8):
                    qv = self.q[4 + u // 4][u % 4]
                    dst = GG[u][i % 2] if i < 5 else TTm[u]
                    self.V(lambda e, u=u, qv=qv, dst=dst, i=i: e.tensor_tensor(out=dst[:], in0=qv[:], in1=GG[u][(i - 1) % 2][:], op=ALU.add), [qv, GG[u][(i - 1) % 2]], [dst])
            AVs = [N("AVs%d" % u, BF16, (128, 64)) for u in range(8)]
            for u in range(8):
                tb, e_ = u // 2, u % 2
                qv = self.q[u // 4][u % 4]
                self.MM(qv[:, 0:64], A4[u][:, 256:384], tokV[:, tb, e_ * 64:(e_ + 1) * 64], True, True, [A4[u], tokV], [qv])
            for u in range(8):
                qv = self.q[u // 4][u % 4]
                self.act(AVs[u], AVs[u][:], qv, qv[:, 0:64], AF.Copy)
            for u in range(8):
                tb, e_ = u // 2, u % 2
                qv = self.q[2 + u // 4][u % 4]
                self.MM(qv[:, 0:64], TTm[u][:], AVs[u][:], True, True, [TTm[u], AVs[u]], [qv])
                qa = self.q[4 + u // 4][u % 4]
                self.MM(qa[:], tokA[:, tb, :], TTm[u][:], True, True, [tokA, TTm[u]], [qa])
            for u in range(8):
                tb, e_ = u // 2, u % 2
                oc = slice(e_ * 64, (e_ + 1) * 64)
                qv = self.q[2 + u // 4][u % 4]
                self.V(lambda e, qv=qv, tb=tb, oc=oc: e.tensor_copy(out=U0[:, tb, oc], in_=qv[:, 0:64]), [qv], [U0])
                qa = self.q[4 + u // 4][u % 4]
                self.A(lambda e, qa=qa, tb=tb, oc=oc: e.activation(out=apT[oc, tb, :], in_=qa[oc, :], func=AF.Copy), [qa], [apT])
            if last and hp == 0:
                self.debug_out("rw_A4", A4[0], A4[0][:], [128, 512], BF16)
                self.debug_out("rw_TT", TTm[0], TTm[0][:], [128, 128], BF16)
                self.debug_out("rw_PT", PT[0], PT[0][:], [128, 128], BF16)
        if getattr(self, "rw_stage", 9) < 3:
            return
        step = 0
        for tb in range(4):
            tbs = slice(tb * 128, (tb + 1) * 128)
            for j in range(2):
                jb = j * 64
                cs = slice(tb * 128 + jb, tb * 128 + jb + 64)
                cidx = tb * 2 + j
                bi = step % 2
                step += 1
                q0, q1, q2, q3 = self.q[bi]
                us = [tb * 2, tb * 2 + 1]
                self.MM(q1[:], apT[:, tb, :], H2b[:], True, False, [apT, H2b], [q1])
                self.MM(q1[:], self.ident_b[:], U0[:, tb, :], False, True, [self.ident_b, U0], [q1])
                self.V(lambda e, q1=q1, jb=jb: e.tensor_copy(out=U_sb[jb:jb + 64, :], in_=q1[jb:jb + 64, :]), [q1], [U_sb])
                for e_ in range(2):
                    oc = slice(e_ * 64, (e_ + 1) * 64)
                    self.MM(q2[:, oc], H2b[:], arT[:, 1, cs], True, False, [H2b, arT], [q2])
                    self.MM(q2[:, oc], U_sb[:], A4[us[e_]][:, 128 + jb:128 + jb + 64], False, False, [U_sb, A4[us[e_]]], [q2])
                    self.MM(q2[:, oc], tokV[:, tb, :], A4[us[e_]][:, 384 + jb:384 + jb + 64], False, True, [tokV, A4[us[e_]]], [q2])
                self.MM(q3[:], tokB[j][:, tb, :], U_sb[:], True, False, [tokB[j], U_sb], [q3])
                self.MM(q3[:], tokK[j][:, tb, :], tokV[:, tb, :], False, True, [tokK[j], tokV], [q3])
                for e_ in range(2):
                    oc = slice(e_ * 64, (e_ + 1) * 64)
                    self.V(lambda e, oc=oc, q3=q3, cidx=cidx: e.scalar_tensor_tensor(out=H2f[oc, oc], in0=H2f[oc, oc], scalar=wl[oc, cidx:cidx + 1], in1=q3[oc, oc],
                                                                                   op0=ALU.mult, op1=ALU.add), [H2f, wl, q3], [H2f])
                    self.A(lambda e, oc=oc: e.activation(out=H2b[oc, oc], in_=H2f[oc, oc], func=AF.Copy), [H2f], [H2b])
                for e_ in range(2):
                    oc = slice(e_ * 64, (e_ + 1) * 64)
                    self.A(lambda e, oc=oc, q2=q2, cs=cs: e.activation(out=y_sb[oc, cs], in_=q2[oc, oc], func=AF.Copy), [q2], [y_sb])
        if last and hp == 0:
            self.debug_out("rw_y", y_sb, y_sb[:], [128, T])
        if getattr(self, "rw_stage", 9) < 4:
            return
        with p.scope():
            ysq, m_sb, yc, m2, var = (N(n) for n in ["ysq", "m_sb", "yc", "m2", "var"])
            ps_m, ps_q = self.next_ps(), self.next_ps()
            self.MM(ps_m[:], self.bd64[:], y_sb[:], True, True, [self.bd64, y_sb], [ps_m])
            self.act(ysq, ysq[:], y_sb, y_sb[:], AF.Square)
            self.MM(ps_q[:], self.bd64[:], ysq[:], True, True, [self.bd64, ysq], [ps_q])
            self.act(m_sb, m_sb[:], ps_m, ps_m[:], AF.Copy)
            self.G(lambda e: e.tensor_tensor(out=yc[:], in0=y_sb[:], in1=m_sb[:], op=ALU.subtract), [y_sb, m_sb], [yc])
            self.G(lambda e: e.tensor_tensor(out=m2[:], in0=m_sb[:], in1=m_sb[:], op=ALU.mult), [m_sb], [m2])
            self.V(lambda e: e.tensor_tensor(out=var[:], in0=ps_q[:], in1=m2[:], op=ALU.subtract), [ps_q, m2], [var])
            self.V(lambda e: e.tensor_scalar(out=var[:], in0=var[:], scalar1=0.0, scalar2=None, op0=ALU.max), [var], [var])
            self.act(var, var[:], var, var[:], AF.Sqrt, bias=self.eps_col[:, 1:2], extra_reads=[self.eps_col])
            self.V(lambda e: e.reciprocal(out=var[:], in_=var[:]), [var], [var])
            self.G(lambda e: e.tensor_tensor(out=yc[:], in0=yc[:], in1=var[:], op=ALU.mult), [yc, var], [yc])
            self.V(lambda e: e.tensor_scalar(out=yc[:], in0=yc[:], scalar1=pv[:, PV["lng"] + hp:PV["lng"] + hp + 1], scalar2=pv[:, PV["lnb"] + hp:PV["lnb"] + hp + 1],
                                             op0=ALU.mult, op1=ALU.add), [yc, pv], [yc])
            self.G(lambda e: e.tensor_tensor(out=yc[:], in0=yc[:], in1=bonus[:], op=ALU.add), [yc, bonus], [yc])
            self.V(lambda e: e.tensor_tensor(out=rwo[:, hp, :], in0=yc[:], in1=g_sb[:], op=ALU.mult), [yc, g_sb], [rwo])


def prep_shared(inp):
    f = lambda a: np.ascontiguousarray(np.asarray(a, dtype=np.float32))
    L = 0
    pv = np.zeros((128, NPV), np.float32)

    def put(name, vec, ntile):
        v = np.asarray(vec, np.float32).reshape(ntile, 128)
        pv[:, PV[name]:PV[name] + ntile] = v.T
    put("mix", inp["mix_norm"][L], 8)
    put("ffn", inp["ffn_norm"][L], 8)
    put("ple", inp["ple_norm"][L], 8)
    put("fin", inp["final_norm"], 8)
    put("mu", inp["mu_shift"][L], 14)
    put("w0", inp["rk_w0"][L], 4)
    put("a0", inp["rk_a0"][L], 4)
    put("kk", inp["rk_k_k"][L], 4)
    put("ka", inp["rk_k_a"][L], 4)
    put("rk", np.asarray(inp["rk_r_k"][L]).reshape(512), 4)
    put("lng", inp["rk_ln_g"][L], 4)
    put("lnb", inp["rk_ln_b"][L], 4)
    put("s5d", np.asarray(inp["s5_d"][L]).reshape(256), 2)
    put("glub", inp["s5_glu_b"][L], 2)
    lre = np.asarray(inp["s5_lam_re"][L], np.float32)
    lim = np.asarray(inp["s5_lam_im"][L], np.float32)
    ldt = np.asarray(inp["s5_log_dt"][L], np.float32)
    for gp in range(8):
        for g2 in range(2):
            g = 2 * gp + g2
            pv[g2 * 64:(g2 + 1) * 64, PV["lre"] + gp] = lre[g]
            pv[g2 * 64:(g2 + 1) * 64, PV["lim"] + gp] = lim[g]
            pv[g2 * 64:(g2 + 1) * 64, PV["ldt"] + gp] = ldt[g]
    bre = np.asarray(inp["s5_b_re"][L], np.float32)
    bim = np.asarray(inp["s5_b_im"][L], np.float32)
    cre = np.asarray(inp["s5_c_re"][L], np.float32)
    cim = np.asarray(inp["s5_c_im"][L], np.float32)
    Bre = np.zeros((256, 512), np.float32)
    Bim = np.zeros((256, 512), np.float32)
    Cre = np.zeros((128, 8, 128), np.float32)
    Cim = np.zeros((128, 8, 128), np.float32)
    for g in range(16):
        col0 = ((g % 8) // 2) * 128 + (g % 2) * 64
        Bre[g * 16:(g + 1) * 16, col0:col0 + 64] = bre[g].T
        Bim[g * 16:(g + 1) * 16, col0:col0 + 64] = bim[g].T
        gp, g2 = g // 2, g % 2
        Cre[g2 * 64:(g2 + 1) * 64, gp, (g % 8) * 16:(g % 8) * 16 + 16] = cre[g].T
        Cim[g2 * 64:(g2 + 1) * 64, gp, (g % 8) * 16:(g % 8) * 16 + 16] = cim[g].T
    lora_wa = np.concatenate([np.asarray(inp["rk_w_up"][L]), np.asarray(inp["rk_a_up"][L])], axis=0)
    wr = np.concatenate([np.asarray(inp["router_group_w"][L]), np.asarray(inp["router_expert_w"][L])], axis=1)
    rb = np.concatenate([np.asarray(inp["router_group_b"][L]), np.asarray(inp["router_expert_b"][L])], axis=0)
    return {
        "pvec": pv, "w_in": f(inp["w_in"][L]), "lora_wa": f(lora_wa), "lora_g": f(inp["rk_g_up"][L]),
        "glu_w": f(inp["s5_glu_w"][L]), "w_ba": f(inp["w_branch_a"][L]), "w_bb": f(inp["w_branch_b"][L]),
        "w_out": f(inp["w_out"][L]), "wr": f(wr), "rbias": f(np.broadcast_to(rb[None, :], (128, 36))),
        "wg": f(inp["exp_w_gate"][L]), "wu": f(inp["exp_w_up"][L]), "wd": f(inp["exp_w_down"][L]),
        "plg": f(inp["ple_gate_w"][L]), "plp": f(inp["ple_proj"][L]),
        "s5bre": Bre, "s5bim": Bim, "s5cre": f(Cre.reshape(128, 1024)), "s5cim": f(Cim.reshape(128, 1024)),
    }


def prep_core(inp, b0, nseq):
    x = np.asarray(inp["x"], np.float32)[b0:b0 + nseq]
    pp = np.asarray(inp["p"], np.float32)[0, b0:b0 + nseq]
    return {"xT": np.ascontiguousarray(x.transpose(0, 2, 1)), "pT": np.ascontiguousarray(pp.transpose(0, 2, 1))}


_CACHE = {}


def kernel(**inputs):
    B, S, _ = inputs["x"].shape
    nseq = B // NCORES
    key = (nseq, S)
    if key not in _CACHE:
        _CACHE[key] = FullBuilder(nseq, S).build()
    nc = _CACHE[key]
    shared = prep_shared(inputs)
    in_maps = []
    for cidx in range(NCORES):
        m = dict(shared)
        m.update(prep_core(inputs, cidx * nseq, nseq))
        in_maps.append(m)
    res = run_bass_kernel_spmd(nc, in_maps, core_ids=list(range(NCORES)))
    out = np.empty((B, S, D), np.float32)
    for cidx in range(NCORES):
        out[cidx * nseq:(cidx + 1) * nseq] = res.results[cidx]["outT"].transpose(0, 2, 1)
    return out
```

```python
import contextlib
import math
import numpy as np
import concourse.bass as bass
import concourse.mybir as mybir
from concourse.bass_utils import run_bass_kernel_spmd

F32 = mybir.dt.float32
BF16 = mybir.dt.bfloat16
AF = mybir.ActivationFunctionType
ALU = mybir.AluOpType
AX = mybir.AxisListType

ENGS = ["tensor", "vector", "scalar", "gpsimd", "sync"]
D = 1024
T = 512
NCORES = 8
CDEC = math.exp(-0.5)

PV = {}
_o = 0
for _n, _w in [("mix", 8), ("ffn", 8), ("ple", 8), ("fin", 8), ("mu", 14), ("w0", 4), ("a0", 4),
               ("kk", 4), ("ka", 4), ("rk", 4), ("lng", 4), ("lnb", 4), ("s5d", 2), ("glub", 2),
               ("lre", 8), ("lim", 8), ("ldt", 8)]:
    PV[_n] = _o
    _o += _w
NPV = _o


class Buf:
    __slots__ = ("name", "t", "last_w", "readers", "dma_sem", "dma_cnt", "aliases", "root")

    def __init__(self, name, t):
        self.name = name
        self.t = t
        self.last_w = None
        self.readers = []
        self.dma_sem = None
        self.dma_cnt = 0
        self.aliases = []
        self.root = None

    def __getitem__(self, k):
        return self.t[k]


class Prog:
    def __init__(self, nc):
        self.nc = nc
        self.root = contextlib.ExitStack()
        self.stacks = [self.root]
        self.scope_bufs = [[]]
        self.ops = {e: [] for e in ENGS}
        self.esem = {}
        self.ecnt = {e: 0 for e in ENGS}
        self.dma_bufs = []
        self.barrier_bufs = []
        for e in ENGS:
            self.esem[e] = self.root.enter_context(nc.semaphore("es_" + e))
        self.dbg = []
        self.waited = {}
        self.stall_fill = True
        self.side_budget = 0
        self.side = None
        self.tick_every = 8
        self._tick_cnt = 0
        self._in_side = False

    def _tick(self, force=False):
        if self.side is None or self._in_side:
            return
        if self.side_budget <= 0:
            return
        if not force:
            self._tick_cnt += 1
            if self._tick_cnt % self.tick_every:
                return
        self.side_budget -= 1
        self._in_side = True
        try:
            next(self.side)
        except StopIteration:
            self.side = None
        finally:
            self._in_side = False

    def drain_side(self):
        if self.side is None:
            return
        self._in_side = True
        try:
            for _ in self.side:
                pass
        finally:
            self._in_side = False
            self.side = None

    def sb(self, name, shape, dtype=F32):
        self.nuid = getattr(self, "nuid", 0) + 1
        name = "s%d_%s" % (self.nuid, name)
        t = self.stacks[-1].enter_context(self.nc.sbuf_tensor(name, list(shape), dtype))
        b = Buf(name, t)
        self.scope_bufs[-1].append(b)
        return b

    def ps_bank(self, name):
        t = self.root.enter_context(self.nc.psum_tensor(name, [128, 512], F32))
        return Buf(name, t)

    def view(self, parent, name, ap):
        b = Buf(name, ap)
        b.aliases.append(parent)
        parent.aliases.append(b)
        return b

    @contextlib.contextmanager
    def scope(self):
        st = contextlib.ExitStack()
        self.stacks.append(st)
        self.scope_bufs.append([])
        try:
            yield
        finally:
            self.barrier()
            self.stacks.pop()
            self.scope_bufs.pop()
            st.close()

    def barrier(self):
        for e in ENGS:
            waits = []
            for o in ENGS:
                if o != e and o != "sync" and self.ecnt[o] > 0:
                    waits.append((self.esem[o], self.ecnt[o]))
            for b in self.barrier_bufs:
                if b.dma_cnt > 0:
                    waits.append((b.dma_sem, b.dma_cnt))
            self.ops[e].append((None, waits, None))

    def _dma_sem(self, b):
        if b.dma_sem is None:
            b.dma_sem = self.root.enter_context(self.nc.semaphore("ds_" + b.name))
            self.dma_bufs.append(b)
        return b.dma_sem

    def _waits(self, eng, reads, writes, dry=False):
        w = {}

        def need(dep):
            if dep is None:
                return
            kind, key, val = dep
            if kind == "eng":
                if key == eng and eng == "tensor":
                    return
                k = ("eng", key)
                sem = self.esem[key]
            else:
                k = ("dma", id(key))
                sem = key.dma_sem
            if w.get(k, (None, 0))[1] < val:
                w[k] = (sem, val)

        for b0 in reads:
            for b in [b0] + b0.aliases:
                need(b.last_w)
        for b0 in writes:
            for b in [b0] + b0.aliases:
                need(b.last_w)
                for r in b.readers:
                    need(r)
        q = eng.split("_")[-1]
        wd = self.waited.setdefault(q, {})
        out = []
        for kk_, (sem, val) in w.items():
            if wd.get(kk_, 0) >= val:
                continue
            if not dry:
                wd[kk_] = val
            out.append((sem, val))
        return out

    def op_silent(self, eng, fn, reads=(), wwait=()):
        assert eng == "tensor"
        reads = [b.root if b.root is not None else b for b in reads]
        wwait = [b.root if b.root is not None else b for b in wwait]
        if self.side is not None and not self._in_side and self.stall_fill:
            if self._waits(eng, reads, wwait, dry=True):
                self._tick(force=True)
        waits = self._waits(eng, reads, wwait)
        self.ops[eng].append((fn, waits, None))

    def op(self, eng, fn, reads=(), writes=()):
        reads = [b.root if b.root is not None else b for b in reads]
        writes = [b.root if b.root is not None else b for b in writes]
        if eng == "tensor" and self.side is not None and not self._in_side and self.stall_fill:
            if self._waits(eng, reads, writes, dry=True):
                self._tick(force=True)
        waits = self._waits(eng, reads, writes)
        self.ecnt[eng] += 1
        val = self.ecnt[eng]
        self.ops[eng].append((fn, waits, (self.esem[eng], 1)))
        dep = ("eng", eng, val)
        for b in reads:
            b.readers.append(dep)
            if len(b.readers) > 16:
                mx = {}
                for r in b.readers:
                    k = (r[0], id(r[1]) if r[0] == "dma" else r[1])
                    if k not in mx or mx[k][2] < r[2]:
                        mx[k] = r
                b.readers = list(mx.values())
        for b in writes:
            b.last_w = dep
            b.readers = []
        self._tick()
        return val

    def dma(self, eng, out_ap, in_ap, reads=(), writes=(), sem_buf=None):
        waits = self._waits("dmaq_" + eng, reads, writes)
        sb_ = sem_buf if sem_buf is not None else writes[0]
        sem = self._dma_sem(sb_)
        sb_.dma_cnt += 16
        val = sb_.dma_cnt

        def fn(e, out_ap=out_ap, in_ap=in_ap):
            return e.dma_start(out=out_ap, in_=in_ap)
        self.ops[eng].append((fn, waits, (sem, 16)))
        dep = ("dma", sb_, val)
        for b in reads:
            b.readers.append(dep)
        for b in writes:
            b.last_w = dep
            b.readers = []
        if sem_buf is not None and not writes:
            sem_buf.last_w = dep

    def final_wait(self, eng, bufs):
        waits = self._waits("final_" + eng, bufs, ())
        self.ops[eng].append((None, waits, None))

    def emit(self):
        with self.nc.Block() as block:
            for e in ENGS:
                ops = self.ops[e]
                if not ops:
                    continue

                def body(eng_obj, ops=ops):
                    for fn, waits, inc in ops:
                        for sem, val in waits:
                            eng_obj.wait_ge(sem, val)
                        if fn is None:
                            continue
                        ins = fn(eng_obj)
                        if inc is not None:
                            ins.then_inc(inc[0], inc[1])
                getattr(block, e)(body)


class Builder:
    def __init__(self, nseq, seq, en_rwkv=True, en_s5=True, en_moe=True, en_ple=True, dbg=()):
        self.nseq, self.seq = nseq, seq
        self.en_rwkv, self.en_s5, self.en_moe, self.en_ple = en_rwkv, en_s5, en_moe, en_ple
        self.dbg_names = set(dbg)
        self.nc = bass.Bass("TRN2", target_bir_lowering=False)
        self.p = Prog(self.nc)
        self.dbg_outs = {}
        self._rr = 0
        self._cnt = 0
        self._grp_reads = {}
        self.half_slices = True
        self.silent_mm = True
        self.tick_rwkv, self.tick_s5, self.tick_merge = 100000, 6, 8
        self.bud_rwkv, self.bud_s5, self.bud_merge = 1000, 1000, 1000

    def uid(self, s):
        self._cnt += 1
        return "%s_%d" % (s, self._cnt)

    def din(self, name, shape, dtype=F32):
        return self.nc.dram_tensor(name, list(shape), dtype, kind="ExternalInput").ap()

    def V(self, fn, reads, writes):
        self.p.op("vector", fn, reads, writes)

    def A(self, fn, reads, writes):
        self.p.op("scalar", fn, reads, writes)

    def G(self, fn, reads, writes):
        self.p.op("gpsimd", fn, reads, writes)

    def MM(self, out_ap, lhsT, rhs, start, stop, reads, writes):
        key = id(writes[0].root if writes[0].root is not None else writes[0])
        if start:
            self._grp_reads = {}
        pend = self._grp_reads.setdefault(key, [])
        if not stop and self.silent_mm:
            if start:
                self.p.op_silent("tensor", lambda e: e.matmul(out_ap, lhsT=lhsT, rhs=rhs, start=start, stop=stop), reads, writes)
            else:
                self.p.op_silent("tensor", lambda e: e.matmul(out_ap, lhsT=lhsT, rhs=rhs, start=start, stop=stop), reads)
            pend.extend(reads)
            return
        allr = list(reads) + [b for b in pend if b not in reads]
        self._grp_reads[key] = []
        self.p.op("tensor", lambda e: e.matmul(out_ap, lhsT=lhsT, rhs=rhs, start=start, stop=stop), allr, writes)

    def act(self, out_b, out_ap, in_b, in_ap, func, bias=None, scale=None, extra_reads=()):
        kw = {}
        if bias is not None:
            kw["bias"] = bias
        if scale is not None:
            kw["scale"] = scale
        self.A(lambda e: e.activation(out=out_ap, in_=in_ap, func=func, **kw), [in_b] + list(extra_reads), [out_b])

    def next_ps(self):
        b = self.psr[self._rr % len(self.psr)]
        self._rr += 1
        return b

    def debug_out(self, name, buf, ap, shape, dtype=F32):
        if name not in self.dbg_names:
            return
        d = self.nc.dram_tensor("dbg_" + name, list(shape), dtype, kind="ExternalOutput").ap()
        db = Buf("dbg_" + name, d)
        self.p.dma("sync", d, ap, reads=[buf], writes=[db])
        self.p.barrier_bufs.append(db)
        self.dbg_outs[name] = db

    def load_w(self, dram_ap, kt, cols, key):
        n = kt * cols
        assert n <= 2048
        if self.p._in_side:
            i = self.NMAIN + (self._wrr_side % (len(self.wbf) - self.NMAIN))
            self._wrr_side += 1
        else:
            i = self._wrr % self.NMAIN
            self._wrr += 1
        wb = self.wbf[i]
        wbv = wb[:, 0:n].rearrange("p (k c) -> p k c", k=kt)
        if key in self.wcache:
            off = self.wcache[key]
            self.p.dma("sync", wb[:, 0:n], self.wscr[:, off:off + n], reads=([] if self.wscr_ready else self.wsb), writes=[wb])
            return wb, wbv
        assert self.wst is not None, "first use of %s after staging was freed" % (key,)
        st = self.wst[self._srr % len(self.wst)]
        self._srr += 1
        stv = st[:, 0:n].rearrange("p (k c) -> p k c", k=kt)
        self.p.dma("sync", stv, dram_ap.rearrange("(k p) c -> p k c", p=128), writes=[st])
        ce = self._crr % 3
        self._crr += 1
        if ce == 0:
            self.G(lambda e: e.tensor_copy(out=wb[:, 0:n], in_=st[:, 0:n]), [st], [wb])
        elif ce == 1:
            self.A(lambda e: e.activation(out=wb[:, 0:n], in_=st[:, 0:n], func=AF.Copy), [st], [wb])
        else:
            self.V(lambda e: e.tensor_copy(out=wb[:, 0:n], in_=st[:, 0:n]), [st], [wb])
        off = self.wscr_off
        self.wscr_off += n
        assert self.wscr_off <= self.WSCR_COLS
        self.wcache[key] = off
        self.p.dma("sync", self.wscr[:, off:off + n], wb[:, 0:n], reads=[wb], writes=[], sem_buf=self.wsb[i])
        return wb, wbv

    def rmsnorm(self, x, gcol, out_b, out_f=None, out_is_f32_only=False):
        p = self.p
        ps = self.next_ps()
        sil_, self.silent_mm = self.silent_mm, False
        for k in range(8):
            sq = self.sqb[k % 2]
            self.act(sq, sq[:], x, x[:, k, :], AF.Square)
            self.MM(ps[:], self.ones_bf[:], sq[:], k == 0, k == 7, [self.ones_bf, sq], [ps])
        self.silent_mm = sil_
        rt = self.rt
        self.act(rt, rt[:], ps, ps[:], AF.Sqrt, bias=self.eps_col[:, 0:1], scale=1.0 / D, extra_reads=[self.eps_col])
        self.V(lambda e: e.reciprocal(out=rt[:], in_=rt[:]), [rt], [rt])
        pv = self.pvec
        for k in range(8):
            if out_f is not None:
                self.V(lambda e, k=k: e.scalar_tensor_tensor(out=out_f[:, k, :], in0=x[:, k, :], scalar=pv[:, gcol + k:gcol + k + 1],
                                                             in1=rt[:], op0=ALU.mult, op1=ALU.mult), [x, pv, rt], [out_f])
                if out_b is not None:
                    self.G(lambda e, k=k: e.tensor_copy(out=out_b[:, k, :], in_=out_f[:, k, :]), [out_f], [out_b])
            else:
                self.V(lambda e, k=k: e.scalar_tensor_tensor(out=out_b[:, k, :], in0=x[:, k, :], scalar=pv[:, gcol + k:gcol + k + 1],
                                                             in1=rt[:], op0=ALU.mult, op1=ALU.mult), [x, pv, rt], [out_b])

    def build(self):
        nc, p = self.nc, self.p
        nseq, seq = self.nseq, self.seq
        nchunk = seq // T
        self.xT = self.din("xT", [nseq, D, seq])
        self.pT = self.din("pT", [nseq, 256, seq])
        self.pvec_d = self.din("pvec", [128, NPV])
        self.w_in = self.din("w_in", [D, 4096])
        self.lora_wa = self.din("lora_wa", [128, 512])
        self.lora_g = self.din("lora_g", [128, 512])
        self.glu_w = self.din("glu_w", [256, 256])
        self.w_ba = self.din("w_ba", [512, D])
        self.w_bb = self.din("w_bb", [256, D])
        self.w_out = self.din("w_out", [D, D])
        self.wr = self.din("wr", [D, 36])
        self.rbias_d = self.din("rbias", [128, 36])
        self.wg = self.din("wg", [32, D, 256])
        self.wu = self.din("wu", [32, D, 256])
        self.wd = self.din("wd", [32, 256, D])
        self.plg = self.din("plg", [D, D])
        self.plp = self.din("plp", [256, D])
        self.s5bre = self.din("s5bre", [256, 512])
        self.s5bim = self.din("s5bim", [256, 512])
        self.s5cre = self.din("s5cre", [128, 8 * 128])
        self.s5cim = self.din("s5cim", [128, 8 * 128])
        self.outT = self.nc.dram_tensor("outT", [nseq, D, seq], F32, kind="ExternalOutput").ap()
        self.out_b = Buf("outT", self.outT)
        p.barrier_bufs.append(self.out_b)

        self.banks = [p.ps_bank("bank%d" % i) for i in range(8)]
        self.psr = self.banks[0:4]
        self.psa = self.banks[4:6]
        self.ps_side = self.banks[6:8]

        self.pvec = p.sb("pvec", [128, NPV])
        self.xs = [p.sb("x0", [128, 8, T])]
        self.x = self.xs[0]
        self.hb = p.sb("hb", [128, 8, T], BF16)
        self.hbm = p.sb("hbm", [128, 8, T], BF16)
        self.sqb = [p.sb("sq%d" % i, [128, T], BF16) for i in range(2)]
        self.rt = p.sb("rt", [128, T])
        self.ones_bf = p.sb("ones_bf", [128, 128], BF16)
        self.eps_col = p.sb("eps_col", [128, 4])
        self.ident_f = p.sb("ident_f", [128, 128])
        self.ident_b = p.sb("ident_b", [128, 128], BF16)
        self.wst = None
        self.wbf = [p.sb("wbf%d" % i, [128, 2048], BF16) for i in range(9)]
        self._wrr = 0
        self._wrr_side = 0
        self.NMAIN = 4
        self._srr = 0
        self._crr = 0
        self.wcache = {}
        self.wscr_off = 0
        self.WSCR_COLS = 262144
        self.wscr = self.nc.dram_tensor("wscr", [128, self.WSCR_COLS], BF16, kind="Internal").ap()
        self.wsb = [Buf("wsb%d" % i, self.wscr) for i in range(len(self.wbf))]
        self.wscr_ready = False
        self.wr_sb = p.sb("wr_sb", [128, 8, 36])
        self.rbias = p.sb("rbias", [128, 36])
        self.combT = p.sb("combT", [32, T], BF16)
        self.m_cb = [p.sb("cb%d" % i, [128, T], BF16) for i in range(2)]
        self.m_sg = [p.sb("sg%d" % i, [128, T], BF16) for i in range(2)]
        self.m_tu = [p.sb("tu%d" % i, [128, T], BF16) for i in range(2)]
        self.m_hid = [p.sb("hid%d" % i, [128, 2, T], BF16) for i in range(4)]

        p.dma("sync", self.pvec[:], self.pvec_d, writes=[self.pvec])
        p.dma("sync", self.wr_sb[:], self.wr.rearrange("(k p) c -> p k c", p=128), writes=[self.wr_sb])
        p.dma("sync", self.rbias[:], self.rbias_d, writes=[self.rbias])
        self.G(lambda e: e.memset(self.ones_bf[:], 1.0), [], [self.ones_bf])
        self.G(lambda e: e.memset(self.eps_col[:, 0:1], 1e-6), [], [self.eps_col])
        self.G(lambda e: e.memset(self.eps_col[:, 1:2], 64e-5), [], [self.eps_col])
        self.G(lambda e: e.memset(self.eps_col[:, 2:3], math.pi / 2), [], [self.eps_col])
        self.G(lambda e: e.memset(self.eps_col[:, 3:4], 0.0), [], [self.eps_col])
        self.G(lambda e: e.memset(self.ident_f[:], 1.0), [], [self.ident_f])
        self.G(lambda e: e.affine_select(out=self.ident_f[:], in_=self.ident_f[:], pattern=[[-1, 128]],
                                         compare_op=ALU.is_equal, fill=0.0, base=0, channel_multiplier=1),
               [self.ident_f], [self.ident_f])
        self.G(lambda e: e.tensor_copy(out=self.ident_b[:], in_=self.ident_f[:]), [self.ident_f], [self.ident_b])

        if self.en_rwkv:
            self.setup_rwkv(0)
        if self.en_s5:
            self.setup_s5(0)

        units = [(s, c) for c in range(nchunk) for s in range(nseq)]

        def load_x(ui):
            s, c = units[ui]
            x = self.xs[ui % 2]
            p.dma("sync", x[:], self.xT[s, :, c * T:(c + 1) * T].rearrange("(k p) t -> p k t", p=128), writes=[x])
            return x

        def do_mixer(ui, x):
            s, c = units[ui]
            self.x = x
            if self.en_rwkv or self.en_s5:
                self.set_seq(s)
                with p.scope():
                    self.mixer(s, c)
            if ui == len(units) - 1:
                self.debug_out("x_mix", x, x[:], [128, 8, T])

        def do_tail(ui, x):
            s, c = units[ui]
            if ui == len(units) - 1:
                self.debug_out("x_moe", x, x[:], [128, 8, T])
            with p.scope():
                if self.en_ple:
                    self.ple(x, s, c)
                self.final(x, s, c, ui == len(units) - 1)

        with p.scope():
            self.wst = [p.sb("wst%d" % i, [128, 2048]) for i in range(2)]
            if self.en_rwkv:
                self.setup_rwkv(1)
            if self.en_s5:
                self.setup_s5(1)
            x0 = load_x(0)
            do_mixer(0, x0)
            if self.en_moe:
                with p.scope():
                    self.moe_head(x0)
                with p.scope():
                    wst0 = self.wst
                    self.wst = wst0 + [p.sb("wstx%d" % i, [128, 2048]) for i in range(6)]
                    nmain0, self.NMAIN = self.NMAIN, len(self.wbf)
                    for _ in self.experts_gen(x0, self.banks):
                        pass
                    self.NMAIN = nmain0
                    self._wrr = 0
                    self.wst = wst0
            do_tail(0, x0)
            p.final_wait("sync", self.wsb)
            self.wscr_ready = True
        self.wst = None
        self.xs.append(p.sb("x1", [128, 8, T]))
        prev = None
        for ui in range(1, len(units)):
            x = load_x(ui)
            if prev is not None and self.en_moe:
                p.side = self.experts_gen(prev[1], self.ps_side)
            do_mixer(ui, x)
            p.drain_side()
            if prev is not None:
                do_tail(prev[0], prev[1])
            if self.en_moe:
                with p.scope():
                    self.moe_head(x)
            prev = (ui, x)
        if prev is not None:
            if self.en_moe:
                for _ in self.experts_gen(prev[1], self.banks):
                    pass
            do_tail(prev[0], prev[1])

        p.final_wait("sync", [self.out_b] + list(self.dbg_outs.values()))
        p.emit()
        return nc

    def set_seq(self, s):
        pass

    def moe_head(self, x):
        p = self.p
        pv = self.pvec
        h2f = p.sb("h2f", [128, 8, T])
        hb = self.hbm
        self.rmsnorm(x, PV["ffn"], hb, out_f=h2f)
        lg = p.sb("lg", [128, 4, 36])
        sm4 = p.sb("sm4", [128, 8, 4])
        gm = p.sb("gm", [128, 4, 4])
        ge = p.sb("ge", [128, 4, 4])
        el = p.sb("el", [128, 4, 8])
        el2 = p.sb("el2", [128, 4, 8])
        t8 = p.sb("t8", [128, 4, 8])
        m1 = p.sb("m1", [128, 4, 8])
        m2 = p.sb("m2", [128, 4, 8])
        cg = p.sb("cg", [128, 4, 8])
        comb = p.sb("comb", [128, 4, 32])
        combT = self.combT
        S = lambda i: sm4[:, i, :]
        Sb = lambda i, n: sm4[:, i, :].unsqueeze(2).to_broadcast([128, 4, n])
        for tb in range(4):
            ps = self.next_ps()
            for k in range(8):
                self.MM(ps[:, 0:36], h2f[:, k, tb * 128:(tb + 1) * 128], self.wr_sb[:, k, :], k == 0, k == 7, [h2f, self.wr_sb], [ps])
            self.V(lambda e, ps=ps, tb=tb: e.tensor_tensor(out=lg[:, tb, :], in0=ps[:, 0:36], in1=self.rbias[:], op=ALU.add), [ps, self.rbias], [lg])
        V = self.V
        V(lambda e: e.tensor_reduce(out=S(0), in_=lg[:, :, 0:4], axis=AX.X, op=ALU.max), [lg], [sm4])
        V(lambda e: e.tensor_tensor(out=gm[:], in0=lg[:, :, 0:4], in1=Sb(0, 4), op=ALU.is_equal), [lg, sm4], [gm])
        V(lambda e: e.tensor_tensor(out=ge[:], in0=lg[:, :, 0:4], in1=Sb(0, 4), op=ALU.subtract), [lg, sm4], [ge])
        self.A(lambda e: e.activation(out=ge[:], in_=ge[:], func=AF.Exp), [ge], [ge])
        V(lambda e: e.tensor_reduce(out=S(1), in_=ge[:], axis=AX.X, op=ALU.add), [ge], [sm4])
        V(lambda e: e.reciprocal(out=S(2), in_=S(1)), [sm4], [sm4])
        V(lambda e: e.tensor_tensor(out=el[:], in0=lg[:, :, 4:12], in1=gm[:, :, 0:1].to_broadcast([128, 4, 8]), op=ALU.mult), [lg, gm], [el])
        for g in range(1, 4):
            V(lambda e, g=g: e.tensor_tensor(out=t8[:], in0=lg[:, :, 4 + 8 * g:12 + 8 * g], in1=gm[:, :, g:g + 1].to_broadcast([128, 4, 8]), op=ALU.mult), [lg, gm], [t8])
            V(lambda e: e.tensor_tensor(out=el[:], in0=el[:], in1=t8[:], op=ALU.add), [el, t8], [el])
        V(lambda e: e.tensor_reduce(out=S(3), in_=el[:], axis=AX.X, op=ALU.max), [el], [sm4])
        V(lambda e: e.tensor_tensor(out=m1[:], in0=el[:], in1=Sb(3, 8), op=ALU.is_equal), [el, sm4], [m1])
        V(lambda e: e.scalar_tensor_tensor(out=el2[:], in0=m1[:], scalar=-1e30, in1=el[:], op0=ALU.mult, op1=ALU.add), [m1, el], [el2])
        V(lambda e: e.tensor_reduce(out=S(4), in_=el2[:], axis=AX.X, op=ALU.max), [el2], [sm4])
        V(lambda e: e.tensor_tensor(out=m2[:], in0=el2[:], in1=Sb(4, 8), op=ALU.is_equal), [el2, sm4], [m2])
        V(lambda e: e.tensor_tensor(out=S(5), in0=S(3), in1=S(4), op=ALU.subtract), [sm4], [sm4])
        self.A(lambda e: e.activation(out=S(5), in_=S(5), func=AF.Sigmoid), [sm4], [sm4])
        V(lambda e: e.tensor_scalar(out=S(6), in0=S(5), scalar1=-1.0, scalar2=1.0, op0=ALU.mult, op1=ALU.add), [sm4], [sm4])
        V(lambda e: e.tensor_tensor(out=S(5), in0=S(5), in1=S(2), op=ALU.mult), [sm4], [sm4])
        V(lambda e: e.tensor_tensor(out=S(6), in0=S(6), in1=S(2), op=ALU.mult), [sm4], [sm4])
        V(lambda e: e.tensor_tensor(out=cg[:], in0=m1[:], in1=Sb(5, 8), op=ALU.mult), [m1, sm4], [cg])
        V(lambda e: e.tensor_tensor(out=t8[:], in0=m2[:], in1=Sb(6, 8), op=ALU.mult), [m2, sm4], [t8])
        V(lambda e: e.tensor_tensor(out=cg[:], in0=cg[:], in1=t8[:], op=ALU.add), [cg, t8], [cg])
        for g in range(4):
            V(lambda e, g=g: e.tensor_tensor(out=comb[:, :, 8 * g:8 * g + 8], in0=cg[:], in1=gm[:, :, g:g + 1].to_broadcast([128, 4, 8]), op=ALU.mult), [cg, gm], [comb])
        ps2 = self.next_ps()
        sil_, self.silent_mm = self.silent_mm, False
        for tb in range(4):
            self.MM(ps2[0:32, tb * 128:(tb + 1) * 128], comb[:, tb, :], self.ident_f[:], True, True, [comb, self.ident_f], [ps2])
        self.silent_mm = sil_
        self.A(lambda e, ps2=ps2: e.activation(out=combT[:], in_=ps2[0:32, :], func=AF.Copy), [ps2], [combT])

    def experts_gen(self, x, banks):
        p = self.p
        hb, combT = self.hbm, self.combT
        cb, sg, tu, hid = self.m_cb, self.m_sg, self.m_tu, self.m_hid
        st = {"rr": 0}

        def nps():
            b = banks[st["rr"] % len(banks)]
            st["rr"] += 1
            return b
        for pr in range(16):
            hds = []
            for el_ in range(2):
                ex = pr * 2 + el_
                psc = nps()
                self.MM(psc[:], self.ident_b[0:32, ex:ex + 1].to_broadcast([32, 128]), combT[:], True, True, [self.ident_b, combT], [psc])
                cbe = cb[ex % 2]
                self.act(cbe, cbe[:], psc, psc[:], AF.Copy)
                wgb, wgv = self.load_w(self.wg[ex], 8, 256, ("wg", ex))
                wub, wuv = self.load_w(self.wu[ex], 8, 256, ("wu", ex))
                hd = hid[ex % 4]
                hds.append(hd)
                yield
                for blk in range(2):
                    pg = nps()
                    for k in range(8):
                        self.MM(pg[:], wgv[:, k, blk * 128:(blk + 1) * 128], hb[:, k, :], k == 0, k == 7, [wgb, hb], [pg])
                        if k == 3 and self.half_slices:
                            yield
                    sgb, tub = sg[blk], tu[blk]
                    self.act(sgb, sgb[:], pg, pg[:], AF.Silu)
                    yield
                    pu = nps()
                    for k in range(8):
                        self.MM(pu[:], wuv[:, k, blk * 128:(blk + 1) * 128], hb[:, k, :], k == 0, k == 7, [wub, hb], [pu])
                        if k == 3 and self.half_slices:
                            yield
                    self.V(lambda e, pu=pu, sgb=sgb, tub=tub: e.tensor_tensor(out=tub[:], in0=pu[:], in1=sgb[:], op=ALU.mult), [pu, sgb], [tub])
                    self.G(lambda e, tub=tub, cbe=cbe, hd=hd, blk=blk: e.tensor_tensor(out=hd[:, blk, :], in0=tub[:], in1=cbe[:], op=ALU.mult), [tub, cbe], [hd])
                    yield
            wds = [self.load_w(self.wd[pr * 2 + el_], 2, 1024, ("wd", pr * 2 + el_)) for el_ in range(2)]
            for db in range(8):
                pd = nps()
                for el_ in range(2):
                    wdb, wdv = wds[el_]
                    for blk in range(2):
                        self.MM(pd[:], wdv[:, blk, db * 128:(db + 1) * 128], hds[el_][:, blk, :], el_ == 0 and blk == 0, el_ == 1 and blk == 1, [wdb, hds[el_]], [pd])
                self.V(lambda e, pd=pd, db=db: e.tensor_tensor(out=x[:, db, :], in0=pd[:], in1=x[:, db, :], op=ALU.add), [pd, x], [x])
                if db % 2 == 1 or self.half_slices:
                    yield

    def ple(self, x, s, c):
        p = self.p
        t0 = c * T
        hb = self.hb
        self.rmsnorm(x, PV["ple"], hb)
        pst = p.sb("pst", [128, 2, T])
        pb = p.sb("pb", [128, 2, T], BF16)
        p.dma("sync", pst[:], self.pT[s, :, t0:t0 + T].rearrange("(k p) t -> p k t", p=128), writes=[pst])
        self.G(lambda e: e.tensor_copy(out=pb[:], in_=pst[:]), [pst], [pb])
        sgp = [p.sb("sgp%d" % i, [128, T]) for i in range(2)]
        tp = [p.sb("tp%d" % i, [128, T]) for i in range(2)]
        for sl in range(4):
            wpb, wpv = self.load_w(self.plp[:, sl * 256:(sl + 1) * 256], 2, 256, ("plp", sl))
            wgb, wgv = self.load_w(self.plg[:, sl * 256:(sl + 1) * 256], 8, 256, ("plg", sl))
            for j in range(2):
                ob = sl * 2 + j
                pg = self.next_ps()
                for k in range(8):
                    self.MM(pg[:], wgv[:, k, j * 128:(j + 1) * 128], hb[:, k, :], k == 0, k == 7, [wgb, hb], [pg])
                pp = self.next_ps()
                for k in range(2):
                    self.MM(pp[:], wpv[:, k, j * 128:(j + 1) * 128], pb[:, k, :], k == 0, k == 1, [wpb, pb], [pp])
                sgb, tpb = sgp[ob % 2], tp[ob % 2]
                self.act(sgb, sgb[:], pg, pg[:], AF.Sigmoid)
                self.V(lambda e, pp=pp, sgb=sgb, tpb=tpb: e.tensor_tensor(out=tpb[:], in0=pp[:], in1=sgb[:], op=ALU.mult), [pp, sgb], [tpb])
                self.G(lambda e, tpb=tpb, ob=ob: e.tensor_tensor(out=x[:, ob, :], in0=x[:, ob, :], in1=tpb[:], op=ALU.add), [tpb, x], [x])

    def final(self, x, s, c, is_last):
        p = self.p
        t0 = c * T
        of = p.sb("of", [128, 8, T])
        self.rmsnorm(x, PV["fin"], None, out_f=of)
        if is_last:
            self.debug_out("x_fin", x, x[:], [128, 8, T])
            self.debug_out("rt_fin", self.rt, self.rt[:], [128, T])
            self.debug_out("of_fin", of, of[:], [128, 8, T])
            self.debug_out("pvec", self.pvec, self.pvec[:], [128, NPV])
        p.dma("sync", self.outT[s, :, t0:t0 + T].rearrange("(k p) t -> p k t", p=128), of[:], reads=[of], writes=[self.out_b])

    def setup_rwkv(self, stage):
        raise NotImplementedError

    def setup_s5(self, stage):
        raise NotImplementedError

    def mixer(self, s, c):
        raise NotImplementedError


TS = 128


class FullBuilder(Builder):
    def make_views(self):
        p = self.p
        self.q = []
        self.h = []
        for i, b in enumerate(self.banks):
            qs = [Buf("q%d_%d" % (i, j), b[:, j * 128:(j + 1) * 128]) for j in range(4)]
            hs = [Buf("h%d_%d" % (i, j), b[:, j * 256:(j + 1) * 256]) for j in range(2)]
            for v in qs + hs:
                v.root = b
            self.q.append(qs)
            self.h.append(hs)

    def setup_rwkv(self, stage):
        p = self.p
        if stage == 0:
            if not hasattr(self, "q"):
                self.make_views()
            self.lw_b = p.sb("lw_b", [128, 512], BF16)
            self.la_b = p.sb("la_b", [128, 512], BF16)
            self.lg_b = p.sb("lg_b", [128, 512], BF16)
            self.hmask = p.sb("hmask", [128, 2])
            self.mask4 = p.sb("mask4", [128, 512], BF16)
            self.m_sl = p.sb("m_sl", [128, 128], BF16)
            self.bd_b = p.sb("bd_b", [128, 128], BF16)
            self.bd64 = p.sb("bd64", [128, 128])
            self.rmask = p.sb("rmask", [128, T], BF16)
            self.onemka = p.sb("onemka", [128, 4])
            self.carrys = [p.sb("carry%d" % s, [128, 14]) for s in range(self.nseq)]
            self.H2fs = [[p.sb("H2f%d_%d" % (s, i), [128, 128]) for i in range(4)] for s in range(self.nseq)]
            self.H2bs = [[p.sb("H2b%d_%d" % (s, i), [128, 128], BF16) for i in range(4)] for s in range(self.nseq)]
            self.carry, self.H2f, self.H2b = self.carrys[0], self.H2fs[0], self.H2bs[0]
            return
        p = self.p
        if not hasattr(self, "q"):
            self.make_views()
        st = self.wst[0]
        p.dma("sync", st[:, 0:512], self.lora_wa, writes=[st])
        self.G(lambda e: e.memset(self.lw_b[:], 0.0), [], [self.lw_b])
        self.G(lambda e: e.memset(self.la_b[:], 0.0), [], [self.la_b])
        self.G(lambda e: e.tensor_copy(out=self.lw_b[0:64, :], in_=st[0:64, 0:512]), [st, self.lw_b], [self.lw_b])
        self.G(lambda e: e.tensor_copy(out=self.la_b[64:128, :], in_=st[64:128, 0:512]), [st, self.la_b], [self.la_b])
        self.G(lambda e: e.memset(self.hmask[:], 0.0), [], [self.hmask])
        self.G(lambda e: e.memset(self.hmask[0:64, 0:1], 1.0), [self.hmask], [self.hmask])
        self.G(lambda e: e.memset(self.hmask[64:128, 1:2], 1.0), [self.hmask], [self.hmask])
        st1 = self.wst[1]
        p.dma("sync", st1[:, 0:512], self.lora_g, writes=[st1])
        self.G(lambda e: e.tensor_copy(out=self.lg_b[:], in_=st1[:, 0:512]), [st1], [self.lg_b])
        mf = p.sb("mf", [128, 128])
        for (cmp_, dsts) in [(ALU.is_gt, [0, 2]), (ALU.is_ge, [1, 3])]:
            self.G(lambda e: e.memset(mf[:], 1.0), [], [mf])
            self.G(lambda e, cmp_=cmp_: e.affine_select(out=mf[:], in_=mf[:], pattern=[[1, 128]], compare_op=cmp_, fill=0.0,
                                                        base=0, channel_multiplier=-1), [mf], [mf])
            self.G(lambda e: e.memset(mf[0:64, 64:128], 0.0), [mf], [mf])
            for d_ in dsts:
                self.G(lambda e, d_=d_: e.tensor_copy(out=self.mask4[:, d_ * 128:(d_ + 1) * 128], in_=mf[:]), [mf], [self.mask4])
        self.G(lambda e: e.memset(mf[:], 1.0), [], [mf])
        self.G(lambda e: e.affine_select(out=mf[:], in_=mf[:], pattern=[[-1, 128]], compare_op=ALU.is_gt, fill=0.0,
                                         base=0, channel_multiplier=1), [mf], [mf])
        self.G(lambda e: e.memset(mf[64:128, 0:64], 0.0), [mf], [mf])
        self.G(lambda e: e.tensor_copy(out=self.m_sl[:], in_=mf[:]), [mf], [self.m_sl])
        self.G(lambda e: e.memset(mf[:], 0.0), [mf], [mf])
        self.G(lambda e: e.memset(mf[0:64, 0:64], 1.0), [mf], [mf])
        self.G(lambda e: e.memset(mf[64:128, 64:128], 1.0), [mf], [mf])
        self.G(lambda e: e.tensor_copy(out=self.bd_b[:], in_=mf[:]), [mf], [self.bd_b])
        self.G(lambda e: e.tensor_scalar(out=self.bd64[:], in0=mf[:], scalar1=1.0 / 64, scalar2=None, op0=ALU.mult), [mf], [self.bd64])
        self.G(lambda e: e.memset(self.rmask[:], 1.0), [], [self.rmask])
        self.G(lambda e: e.memset(self.rmask[:].rearrange("p (c l) -> p c l", l=64)[:, :, 0:1], 0.0), [self.rmask], [self.rmask])
        pv = self.pvec
        self.V(lambda e: e.tensor_scalar(out=self.onemka[:], in0=pv[:, PV["ka"]:PV["ka"] + 4], scalar1=-1.0, scalar2=1.0,
                                         op0=ALU.mult, op1=ALU.add), [pv], [self.onemka])


    def setup_s5(self, stage):
        p = self.p
        if stage == 0:
            if not hasattr(self, "q"):
                self.make_views()
            self.cosT = p.sb("cosT", [128, 8, TS])
            self.sinT = p.sb("sinT", [128, 8, TS])
            self.rho = p.sb("rho", [128, 8])
            self.cTs = p.sb("cTs", [128, 8])
            self.sTs = p.sb("sTs", [128, 8])
            self.nsTs = p.sb("nsTs", [128, 8])
            self.cwre = p.sb("cwre", [128, 8, 128], BF16)
            self.cwimn = p.sb("cwimn", [128, 8, 128], BF16)
            self.bre_b = p.sb("bre_b", [128, 2, 512], BF16)
            self.bim_b = p.sb("bim_b", [128, 2, 512], BF16)
            self.glu_b16 = p.sb("glu_b16", [128, 2, 256], BF16)
            self.s5c_re = p.sb("s5c_re", [128, 8])
            self.s5c_im = p.sb("s5c_im", [128, 8])
            self.s5cars = [p.sb("s5car%d" % s, [128, 2, 8]) for s in range(self.nseq)]
            self.s5car = self.s5cars[0]
            return
        p = self.p
        if not hasattr(self, "q"):
            self.make_views()
        pv = self.pvec
        for (dr, dst) in [(self.s5bre, self.bre_b), (self.s5bim, self.bim_b)]:
            i = self._srr % 2
            self._srr += 1
            st = self.wst[i]
            p.dma("sync", st[:, 0:1024].rearrange("p (k c) -> p k c", k=2), dr.rearrange("(k p) c -> p k c", p=128), writes=[st])
            self.G(lambda e, st=st, dst=dst: e.tensor_copy(out=dst[:].rearrange("p k c -> p (k c)"), in_=st[:, 0:1024]), [st], [dst])
        i = self._srr % 2
        self._srr += 1
        st = self.wst[i]
        p.dma("sync", st[:, 0:512].rearrange("p (k c) -> p k c", k=2), self.glu_w.rearrange("(k p) c -> p k c", p=128), writes=[st])
        self.G(lambda e, st=st: e.tensor_copy(out=self.glu_b16[:].rearrange("p k c -> p (k c)"), in_=st[:, 0:512]), [st], [self.glu_b16])
        with p.scope():
            cre = p.sb("cre", [128, 8, 128])
            cim = p.sb("cim", [128, 8, 128])
            tt = p.sb("s5tt", [128, 16, 8])
            big1 = p.sb("big1", [128, 8, 128])
            big2 = p.sb("big2", [128, 8, 128])
            st0, st1 = self.wst[0], self.wst[1]
            self._srr += 2
            p.dma("sync", st0[:, 0:1024], self.s5cre, writes=[st0])
            p.dma("sync", st1[:, 0:1024], self.s5cim, writes=[st1])
            self.V(lambda e: e.tensor_copy(out=cre[:].rearrange("p a b -> p (a b)"), in_=st0[:, 0:1024]), [st0], [cre])
            self.V(lambda e: e.tensor_copy(out=cim[:].rearrange("p a b -> p (a b)"), in_=st1[:, 0:1024]), [st1], [cim])
            lre = pv[:, PV["lre"]:PV["lre"] + 8]
            lim = pv[:, PV["lim"]:PV["lim"] + 8]
            ldt = pv[:, PV["ldt"]:PV["ldt"] + 8]
            t = lambda i: tt[:, i, :]
            TT = lambda fn: self.V(fn, [tt, pv, self.eps_col], [tt])
            self.A(lambda e: e.activation(out=t(0), in_=ldt, func=AF.Exp), [pv], [tt])
            TT(lambda e: e.tensor_tensor(out=t(1), in0=lre, in1=t(0), op=ALU.mult))
            TT(lambda e: e.tensor_tensor(out=t(2), in0=lim, in1=t(0), op=ALU.mult))
            self.A(lambda e: e.activation(out=self.rho[:], in_=t(1), func=AF.Exp), [tt], [self.rho])
            self.A(lambda e: e.activation(out=t(3), in_=t(2), func=AF.Sin, scale=1.0 / 16), [tt], [tt])
            self.A(lambda e: e.activation(out=t(4), in_=t(2), func=AF.Sin, scale=-1.0 / 16, bias=self.eps_col[:, 2:3]), [tt, self.eps_col], [tt])

            def square(ci, si):
                TT(lambda e: e.tensor_tensor(out=t(5), in0=t(ci), in1=t(ci), op=ALU.mult))
                TT(lambda e: e.tensor_tensor(out=t(6), in0=t(si), in1=t(si), op=ALU.mult))
                TT(lambda e: e.tensor_tensor(out=t(7), in0=t(ci), in1=t(si), op=ALU.mult))
                TT(lambda e: e.tensor_tensor(out=t(ci), in0=t(5), in1=t(6), op=ALU.subtract))
                TT(lambda e: e.tensor_scalar(out=t(si), in0=t(7), scalar1=2.0, scalar2=None, op0=ALU.mult))
            for _ in range(4):
                square(4, 3)
            TT(lambda e: e.tensor_tensor(out=t(8), in0=self.rho[:], in1=t(4), op=ALU.mult))
            TT(lambda e: e.tensor_tensor(out=t(9), in0=self.rho[:], in1=t(3), op=ALU.mult))
            self.V(lambda e: e.tensor_scalar(out=t(8), in0=t(8), scalar1=-1.0, scalar2=None, op0=ALU.add), [tt, self.rho], [tt])
            TT(lambda e: e.tensor_tensor(out=t(10), in0=lre, in1=lre, op=ALU.mult))
            TT(lambda e: e.tensor_tensor(out=t(11), in0=lim, in1=lim, op=ALU.mult))
            TT(lambda e: e.tensor_tensor(out=t(10), in0=t(10), in1=t(11), op=ALU.add))
            TT(lambda e: e.reciprocal(out=t(10), in_=t(10)))
            TT(lambda e: e.tensor_tensor(out=t(11), in0=t(8), in1=lre, op=ALU.mult))
            TT(lambda e: e.tensor_tensor(out=t(12), in0=t(9), in1=lim, op=ALU.mult))
            TT(lambda e: e.tensor_tensor(out=t(11), in0=t(11), in1=t(12), op=ALU.add))
            self.V(lambda e: e.tensor_tensor(out=self.s5c_re[:], in0=t(11), in1=t(10), op=ALU.mult), [tt], [self.s5c_re])
            TT(lambda e: e.tensor_tensor(out=t(11), in0=t(9), in1=lre, op=ALU.mult))
            TT(lambda e: e.tensor_tensor(out=t(12), in0=t(8), in1=lim, op=ALU.mult))
            TT(lambda e: e.tensor_tensor(out=t(11), in0=t(11), in1=t(12), op=ALU.subtract))
            self.V(lambda e: e.tensor_tensor(out=self.s5c_im[:], in0=t(11), in1=t(10), op=ALU.mult), [tt], [self.s5c_im])
            cr_b = self.s5c_re[:].unsqueeze(2).to_broadcast([128, 8, 128])
            ci_b = self.s5c_im[:].unsqueeze(2).to_broadcast([128, 8, 128])
            self.V(lambda e: e.tensor_tensor(out=big1[:], in0=cre[:], in1=cr_b, op=ALU.mult), [cre, self.s5c_re], [big1])
            self.V(lambda e: e.tensor_tensor(out=big2[:], in0=cim[:], in1=ci_b, op=ALU.mult), [cim, self.s5c_im], [big2])
            self.V(lambda e: e.tensor_tensor(out=self.cwre[:], in0=big1[:], in1=big2[:], op=ALU.subtract), [big1, big2], [self.cwre])
            self.V(lambda e: e.tensor_tensor(out=big1[:], in0=cre[:], in1=ci_b, op=ALU.mult), [cre, self.s5c_im], [big1])
            self.V(lambda e: e.tensor_tensor(out=big2[:], in0=cim[:], in1=cr_b, op=ALU.mult), [cim, self.s5c_re], [big2])
            self.V(lambda e: e.scalar_tensor_tensor(out=self.cwimn[:], in0=big1[:], scalar=-1.0, in1=big2[:], op0=ALU.mult, op1=ALU.subtract),
                   [big1, big2], [self.cwimn])
            cosT, sinT = self.cosT, self.sinT
            self.V(lambda e: e.memset(cosT[:, :, 0:1], 1.0), [], [cosT])
            self.V(lambda e: e.memset(sinT[:, :, 0:1], 0.0), [], [sinT])
            L = 1
            tmpa = p.sb("tmpa", [128, 8, 128])
            tmpb = p.sb("tmpb", [128, 8, 128])
            while L < TS:
                pc = t(4).unsqueeze(2).to_broadcast([128, 8, L])
                ps_ = t(3).unsqueeze(2).to_broadcast([128, 8, L])
                self.V(lambda e, L=L, pc=pc: e.tensor_tensor(out=tmpa[:, :, 0:L], in0=cosT[:, :, 0:L], in1=pc, op=ALU.mult), [cosT, tt], [tmpa])
                self.V(lambda e, L=L, ps_=ps_: e.tensor_tensor(out=tmpb[:, :, 0:L], in0=sinT[:, :, 0:L], in1=ps_, op=ALU.mult), [sinT, tt], [tmpb])
                self.V(lambda e, L=L: e.tensor_tensor(out=cosT[:, :, L:2 * L], in0=tmpa[:, :, 0:L], in1=tmpb[:, :, 0:L], op=ALU.subtract), [tmpa, tmpb, cosT], [cosT])
                self.V(lambda e, L=L, ps_=ps_: e.tensor_tensor(out=tmpa[:, :, 0:L], in0=cosT[:, :, 0:L], in1=ps_, op=ALU.mult), [cosT, tt], [tmpa])
                self.V(lambda e, L=L, pc=pc: e.tensor_tensor(out=tmpb[:, :, 0:L], in0=sinT[:, :, 0:L], in1=pc, op=ALU.mult), [sinT, tt], [tmpb])
                self.V(lambda e, L=L: e.tensor_tensor(out=sinT[:, :, L:2 * L], in0=tmpa[:, :, 0:L], in1=tmpb[:, :, 0:L], op=ALU.add), [tmpa, tmpb, sinT], [sinT])
                square(4, 3)
                L *= 2
            self.V(lambda e: e.tensor_copy(out=self.cTs[:], in_=t(4)), [tt], [self.cTs])
            self.V(lambda e: e.tensor_copy(out=self.sTs[:], in_=t(3)), [tt], [self.sTs])
            self.V(lambda e: e.tensor_scalar(out=self.nsTs[:], in0=t(3), scalar1=-1.0, scalar2=None, op0=ALU.mult), [tt], [self.nsTs])
        self.debug_out("cosT", self.cosT, self.cosT[:], [128, 8, TS])
        self.debug_out("sinT", self.sinT, self.sinT[:], [128, 8, TS])
        self.debug_out("cwre", self.cwre, self.cwre[:], [128, 8, 128], BF16)


    def set_seq(self, s):
        if self.en_rwkv:
            self.carry, self.H2f, self.H2b = self.carrys[s], self.H2fs[s], self.H2bs[s]
        if self.en_s5:
            self.s5car = self.s5cars[s]

    def inproj_block(self, blk, dst_b, dst_ap, first_chunk):
        p = self.p
        hb, pv = self.hb, self.pvec
        wb, wv = self.load_w(self.w_in[:, blk * 128:(blk + 1) * 128], 8, 128, ("win", blk))
        ps = self.next_ps()
        for k in range(8):
            self.MM(ps[:], wv[:, k, :], hb[:, k, :], k == 0, k == 7, [wb, hb], [ps])
        raw = self.rawb[self._rawi % 2]
        tmp = self.shtmp[0]
        self._rawi += 1
        carry = self.carry
        self.act(raw, raw[:, 1:T + 1], ps, ps[:], AF.Copy)
        self.V(lambda e: e.tensor_copy(out=raw[:, 0:1], in_=carry[:, blk:blk + 1]), [carry, raw], [raw])
        self.V(lambda e: e.tensor_copy(out=carry[:, blk:blk + 1], in_=raw[:, T:T + 1]), [raw, carry], [carry])
        self.V(lambda e: e.tensor_tensor(out=tmp[:], in0=raw[:, 0:T], in1=raw[:, 1:T + 1], op=ALU.subtract), [raw], [tmp])
        self.V(lambda e: e.scalar_tensor_tensor(out=dst_ap, in0=tmp[:], scalar=pv[:, PV["mu"] + blk:PV["mu"] + blk + 1], in1=raw[:, 1:T + 1],
                                                op0=ALU.mult, op1=ALU.add), [tmp, pv, raw], [dst_b])
        if blk == 0:
            self.debug_out("ip_raw", raw, raw[:], [128, T + 1])
            self.debug_out("ip_tmp", tmp, tmp[:], [128, T])
            self.debug_out("ip_dst", dst_b, dst_ap, [128, T])
            self.debug_out("ip_w", wb, wb[:, 0:1024], [128, 1024], BF16)
            self.debug_out("ip_hb", hb, hb[:], [128, 8, T], BF16)

    def mixer(self, s, c):
        p = self.p
        x, hb, pv = self.x, self.hb, self.pvec
        last = (s == self.nseq - 1 and c == self.seq // T - 1)
        self.rmsnorm(x, PV["mix"], hb)
        self.rwo = p.sb("rwo", [128, 4, T], BF16)
        self.s5o = p.sb("s5o", [128, 2, T], BF16)
        self.rawb = [p.sb("raw%d" % i, [128, T + 1]) for i in range(2)]
        self.shtmp = [p.sb("shtmp%d" % i, [128, T]) for i in range(1)]
        self._rawi = 0
        if self.en_rwkv:
            if c == 0:
                carry_, H2f_, H2b_ = self.carry, self.H2f, self.H2b
                self.G(lambda e: e.memset(carry_[:], 0.0), [carry_], [carry_])
                for hp in range(4):
                    self.G(lambda e, hp=hp: e.memset(H2f_[hp][:], 0.0), [H2f_[hp]], [H2f_[hp]])
                    self.G(lambda e, hp=hp: e.memset(H2b_[hp][:], 0.0), [H2b_[hp]], [H2b_[hp]])
            if getattr(self, "rw_stage", 9) < 9:
                rwo__ = self.rwo
                self.G(lambda e: e.memset(rwo__[:], 0.0), [], [rwo__])
            self.twd = p.sb("twd", [128, T], BF16)
            self.sgd = p.sb("sgd", [128, T], BF16)
            with p.scope():
                lo12 = p.sb("lo12", [128, T])
                lo13 = p.sb("lo13", [128, T])
                self.inproj_block(12, lo12, lo12[:], c == 0)
                self.inproj_block(13, lo13, lo13[:], c == 0)
                twd_, sgd_ = self.twd, self.sgd
                self.A(lambda e: e.activation(out=twd_[0:64, :], in_=lo12[0:64, :], func=AF.Tanh), [lo12], [twd_])
                self.A(lambda e: e.activation(out=twd_[64:128, :], in_=lo12[64:128, :], func=AF.Copy), [lo12], [twd_])
                self.A(lambda e: e.activation(out=sgd_[:], in_=lo13[:], func=AF.Sigmoid), [lo13], [sgd_])
            p.tick_every = self.tick_rwkv
            p.side_budget = 0
            for hp in range(4):
                p.side_budget += self.bud_rwkv
                with p.scope():
                    self.rwkv_hp(s, c, hp, last)
        else:
            rwo_ = self.rwo
            self.G(lambda e: e.memset(rwo_[:], 0.0), [], [rwo_])
        p.tick_every = self.tick_s5
        p.side_budget += self.bud_s5
        if self.en_s5:
            with p.scope():
                self.s5(s, c, last)
        else:
            s5o_ = self.s5o
            self.G(lambda e: e.memset(s5o_[:], 0.0), [], [s5o_])
        p.tick_every = self.tick_merge
        p.side_budget += 1000
        with p.scope():
            self.merge(s, c, last)

    def s5(self, s, c, last):
        p = self.p
        hb, pv = self.hb, self.pvec
        uf = p.sb("uf", [128, 2, T])
        ub = p.sb("ub", [128, 2, T], BF16)
        zb = p.sb("zb", [128, 2, T], BF16)
        wb, wv = self.load_w(self.w_in[:, 1792:2048], 8, 256, ("win_s5", 0))
        for j in range(2):
            ps = self.next_ps()
            for k in range(8):
                self.MM(ps[:], wv[:, k, j * 128:(j + 1) * 128], hb[:, k, :], k == 0, k == 7, [wb, hb], [ps])
            self.act(uf, uf[:, j, :], ps, ps[:], AF.Copy)
            self.G(lambda e, j=j: e.tensor_copy(out=ub[:, j, :], in_=uf[:, j, :]), [uf], [ub])
        car = self.s5car
        if c == 0:
            self.G(lambda e: e.memset(car[:], 0.0), [car], [car])
        tn = ["t1", "t2", "t3", "t4", "wre", "wim", "zre", "zim", "u1", "u2"]
        tb_ = {n: [p.sb(n + "_%d" % i, [128, TS]) for i in range(2)] for n in tn}
        xre = [p.sb("xre%d" % i, [128, TS], BF16) for i in range(2)]
        xim = [p.sb("xim%d" % i, [128, TS], BF16) for i in range(2)]
        sm = p.sb("s5sm", [128, 8])
        ysb = p.sb("ysb", [128, TS])
        it = 0
        for sc in range(T // TS):
            off = sc * TS
            for gp in range(8):
                kt, gpl = gp // 4, gp % 4
                i = it % 2
                it += 1
                B = {n: tb_[n][i] for n in tn}
                bank = self.next_ps()
                hre, him = None, None
                bi = self.banks.index(bank)
                hre, him = self.h[bi][0], self.h[bi][1]
                self.MM(bank[:, 0:TS], self.bre_b[:, kt, gpl * 128:(gpl + 1) * 128], ub[:, kt, off:off + TS], True, True, [self.bre_b, ub], [bank])
                self.MM(bank[:, TS:2 * TS], self.bim_b[:, kt, gpl * 128:(gpl + 1) * 128], ub[:, kt, off:off + TS], True, True, [self.bim_b, ub], [bank])
                cg_, sg_ = self.cosT[:, gp, :], self.sinT[:, gp, :]
                hre, him = bank, bank
                self.V(lambda e, B=B, bank=bank, cg_=cg_: e.tensor_tensor(out=B["t1"][:], in0=bank[:, 0:TS], in1=cg_, op=ALU.mult), [bank, self.cosT], [B["t1"]])
                self.V(lambda e, B=B, bank=bank, sg_=sg_: e.tensor_tensor(out=B["t2"][:], in0=bank[:, TS:2 * TS], in1=sg_, op=ALU.mult), [bank, self.sinT], [B["t2"]])
                self.V(lambda e, B=B, bank=bank, cg_=cg_: e.tensor_tensor(out=B["t3"][:], in0=bank[:, TS:2 * TS], in1=cg_, op=ALU.mult), [bank, self.cosT], [B["t3"]])
                self.V(lambda e, B=B, bank=bank, sg_=sg_: e.tensor_tensor(out=B["t4"][:], in0=bank[:, 0:TS], in1=sg_, op=ALU.mult), [bank, self.sinT], [B["t4"]])
                self.V(lambda e, B=B: e.tensor_tensor(out=B["wre"][:], in0=B["t1"][:], in1=B["t2"][:], op=ALU.add), [B["t1"], B["t2"]], [B["wre"]])
                self.V(lambda e, B=B: e.tensor_tensor(out=B["wim"][:], in0=B["t3"][:], in1=B["t4"][:], op=ALU.subtract), [B["t3"], B["t4"]], [B["wim"]])
                rb = self.rho[:, gp:gp + 1].to_broadcast([128, TS])
                self.V(lambda e, B=B, rb=rb, gp=gp: e.tensor_tensor_scan(out=B["zre"][:], data0=rb, data1=B["wre"][:], initial=car[:, 0, gp:gp + 1],
                                                                        op0=ALU.mult, op1=ALU.add), [self.rho, B["wre"], car], [B["zre"]])
                self.V(lambda e, B=B, rb=rb, gp=gp: e.tensor_tensor_scan(out=B["zim"][:], data0=rb, data1=B["wim"][:], initial=car[:, 1, gp:gp + 1],
                                                                        op0=ALU.mult, op1=ALU.add), [self.rho, B["wim"], car], [B["zim"]])
                zr, zi = B["zre"], B["zim"]
                cT, sT, nsT = self.cTs[:, gp:gp + 1], self.sTs[:, gp:gp + 1], self.nsTs[:, gp:gp + 1]
                self.V(lambda e, zr=zr, cT=cT: e.tensor_scalar(out=sm[:, 0:1], in0=zr[:, TS - 1:TS], scalar1=cT, scalar2=None, op0=ALU.mult), [zr, self.cTs, sm], [sm])
                self.V(lambda e, zi=zi, cT=cT: e.tensor_scalar(out=sm[:, 1:2], in0=zi[:, TS - 1:TS], scalar1=cT, scalar2=None, op0=ALU.mult), [zi, self.cTs, sm], [sm])
                self.V(lambda e, zi=zi, nsT=nsT, gp=gp: e.scalar_tensor_tensor(out=car[:, 0, gp:gp + 1], in0=zi[:, TS - 1:TS], scalar=nsT, in1=sm[:, 0:1], op0=ALU.mult, op1=ALU.add),
                       [zi, self.nsTs, sm, car], [car])
                self.V(lambda e, zr=zr, sT=sT, gp=gp: e.scalar_tensor_tensor(out=car[:, 1, gp:gp + 1], in0=zr[:, TS - 1:TS], scalar=sT, in1=sm[:, 1:2], op0=ALU.mult, op1=ALU.add),
                       [zr, self.sTs, sm, car], [car])
                xr, xi = xre[i], xim[i]
                self.G(lambda e, B=B, cg_=cg_: e.tensor_tensor(out=B["u1"][:], in0=B["zre"][:], in1=cg_, op=ALU.mult), [B["zre"], self.cosT], [B["u1"]])
                self.G(lambda e, B=B, sg_=sg_: e.tensor_tensor(out=B["u2"][:], in0=B["zim"][:], in1=sg_, op=ALU.mult), [B["zim"], self.sinT], [B["u2"]])
                self.G(lambda e, B=B, xr=xr: e.tensor_tensor(out=xr[:], in0=B["u1"][:], in1=B["u2"][:], op=ALU.subtract), [B["u1"], B["u2"]], [xr])
                self.V(lambda e, B=B, sg_=sg_: e.tensor_tensor(out=B["t1"][:], in0=B["zre"][:], in1=sg_, op=ALU.mult), [B["zre"], self.sinT], [B["t1"]])
                self.V(lambda e, B=B, cg_=cg_: e.tensor_tensor(out=B["t2"][:], in0=B["zim"][:], in1=cg_, op=ALU.mult), [B["zim"], self.cosT], [B["t2"]])
                self.V(lambda e, B=B, xi=xi: e.tensor_tensor(out=xi[:], in0=B["t1"][:], in1=B["t2"][:], op=ALU.add), [B["t1"], B["t2"]], [xi])
                if last and sc == 1 and gp == 0:
                    self.debug_out("s5_zre", B["zre"], B["zre"][:], [128, TS])
                    self.debug_out("s5_xre", xr, xr[:], [128, TS], BF16)
                pa = self.psa[kt]
                self.MM(pa[:, 0:TS], self.cwre[:, gp, :], xr[:], gpl == 0, False, [self.cwre, xr], [pa])
                self.MM(pa[:, 0:TS], self.cwimn[:, gp, :], xi[:], False, gpl == 3, [self.cwimn, xi], [pa])
                if gpl == 3:
                    self.V(lambda e, kt=kt, pa=pa, off=off: e.scalar_tensor_tensor(out=ysb[:], in0=uf[:, kt, off:off + TS], scalar=pv[:, PV["s5d"] + kt:PV["s5d"] + kt + 1],
                                                                                  in1=pa[:, 0:TS], op0=ALU.mult, op1=ALU.add), [uf, pv, pa], [ysb])
                    if last and sc == 1 and kt == 0:
                        self.debug_out("s5_y", ysb, ysb[:], [128, TS])
                    self.A(lambda e, kt=kt, off=off: e.activation(out=zb[:, kt, off:off + TS], in_=ysb[:], func=AF.Gelu), [ysb], [zb])
        sgl = p.sb("sgl", [128, T])
        for ob in range(2):
            ps = self.next_ps()
            for k in range(2):
                self.MM(ps[:], self.glu_b16[:, k, ob * 128:(ob + 1) * 128], zb[:, k, :], k == 0, k == 1, [self.glu_b16, zb], [ps])
            self.A(lambda e, ps=ps, ob=ob: e.activation(out=sgl[:], in_=ps[:], func=AF.Sigmoid, bias=pv[:, PV["glub"] + ob:PV["glub"] + ob + 1]), [ps, pv], [sgl])
            s5o_ = self.s5o
            self.V(lambda e, ob=ob: e.tensor_tensor(out=s5o_[:, ob, :], in0=zb[:, ob, :], in1=sgl[:], op=ALU.mult), [zb, sgl], [s5o_])
        if last:
            self.debug_out("s5o", self.s5o, self.s5o[:], [128, 2, T], BF16)

    def merge(self, s, c, last):
        p = self.p
        hb, x = self.hb, self.x
        mg = p.sb("mg", [128, 8, T], BF16)
        ga = [p.sb("ga%d" % i, [128, T], BF16) for i in range(2)]
        gb = [p.sb("gb%d" % i, [128, T], BF16) for i in range(2)]
        t1 = [p.sb("mt1_%d" % i, [128, T]) for i in range(2)]
        t2 = [p.sb("mt2_%d" % i, [128, T]) for i in range(2)]
        for ob in range(8):
            i = ob % 2
            wab, wav = self.load_w(self.w_in[:, 2048 + ob * 128:2048 + (ob + 1) * 128], 8, 128, ("win_ga", ob))
            pg = self.next_ps()
            for k in range(8):
                self.MM(pg[:], wav[:, k, :], hb[:, k, :], k == 0, k == 7, [wab, hb], [pg])
            self.act(ga[i], ga[i][:], pg, pg[:], AF.Sigmoid)
            wbb_, wbv = self.load_w(self.w_in[:, 3072 + ob * 128:3072 + (ob + 1) * 128], 8, 128, ("win_gb", ob))
            pg2 = self.next_ps()
            for k in range(8):
                self.MM(pg2[:], wbv[:, k, :], hb[:, k, :], k == 0, k == 7, [wbb_, hb], [pg2])
            self.act(gb[i], gb[i][:], pg2, pg2[:], AF.Sigmoid)
            w3b, w3v = self.load_w(self.w_ba[:, ob * 128:(ob + 1) * 128], 4, 128, ("wba", ob))
            pa = self.next_ps()
            for k in range(4):
                self.MM(pa[:], w3v[:, k, :], self.rwo[:, k, :], k == 0, k == 3, [w3b, self.rwo], [pa])
            self.V(lambda e, pa=pa, i=i: e.tensor_tensor(out=t1[i][:], in0=pa[:], in1=ga[i][:], op=ALU.mult), [pa, ga[i]], [t1[i]])
            w4b, w4v = self.load_w(self.w_bb[:, ob * 128:(ob + 1) * 128], 2, 128, ("wbb", ob))
            pb_ = self.next_ps()
            for k in range(2):
                self.MM(pb_[:], w4v[:, k, :], self.s5o[:, k, :], k == 0, k == 1, [w4b, self.s5o], [pb_])
            self.V(lambda e, pb_=pb_, i=i: e.tensor_tensor(out=t2[i][:], in0=pb_[:], in1=gb[i][:], op=ALU.mult), [pb_, gb[i]], [t2[i]])
            self.G(lambda e, i=i, ob=ob: e.tensor_tensor(out=mg[:, ob, :], in0=t1[i][:], in1=t2[i][:], op=ALU.add), [t1[i], t2[i]], [mg])
        if last:
            self.debug_out("mg", mg, mg[:], [128, 8, T], BF16)
            self.debug_out("rwo", self.rwo, self.rwo[:], [128, 4, T], BF16)
        for sl in range(4):
            wob, wov = self.load_w(self.w_out[:, sl * 256:(sl + 1) * 256], 8, 256, ("wout", sl))
            for j in range(2):
                ob = sl * 2 + j
                ps = self.next_ps()
                for k in range(8):
                    self.MM(ps[:], wov[:, k, j * 128:(j + 1) * 128], mg[:, k, :], k == 0, k == 7, [wob, mg], [ps])
                self.V(lambda e, ps=ps, ob=ob: e.tensor_tensor(out=x[:, ob, :], in0=ps[:], in1=x[:, ob, :], op=ALU.add), [ps, x], [x])

    def rwkv_hp(self, s, c, hp, last):
        p = self.p
        pv, hb = self.pvec, self.hb
        twd, sgd, rwo = self.twd, self.sgd, self.rwo
        H2f, H2b = self.H2f[hp], self.H2b[hp]
        hmask = self.hmask
        N = lambda nm, dt=F32, shape=(128, T): p.sb(nm, list(shape), dt)
        arT = N("arT", BF16, (128, 2, T))
        bT = N("bT", BF16)
        pad = {(q, e): N("pad%s%d" % (q, e), BF16) for q in "abk" for e in range(2)}
        tokV = N("tokV", BF16, (128, 4, 128))
        tokA = N("tokA", BF16, (128, 4, 128))
        apT = N("apT", BF16, (128, 4, 128))
        U0 = N("U0", BF16, (128, 4, 128))
        tokB = [N("tokB%d" % j, BF16, (128, 4, 128)) for j in range(2)]
        tokK = [N("tokK%d" % j, BF16, (128, 4, 128)) for j in range(2)]
        A4 = [N("A4_%d" % u, BF16, (128, 512)) for u in range(8)]
        TTm = [N("TT_%d" % u, BF16, (128, 128)) for u in range(8)]
        wl = N("wl", F32, (128, 8))
        g_sb = N("g_sb")
        bonus = N("bonus")
        y_sb = N("y_sb")
        U_sb = N("U_sb", BF16, (128, 128))
        self.G(lambda e: e.memset(U_sb[:], 0.0), [], [U_sb])
        with p.scope():
            rS, kS, vS = N("rS"), N("kS"), N("vS")
            self.inproj_block(hp, rS, rS[:], c == 0)
            self.inproj_block(4 + hp, kS, kS[:], c == 0)
            self.inproj_block(8 + hp, vS, vS[:], c == 0)
            cs_ = slice(hp * 128, (hp + 1) * 128)
            ps_w, ps_a, ps_g = self.next_ps(), self.next_ps(), self.next_ps()
            self.MM(ps_w[:], self.lw_b[:, cs_], twd[:], True, True, [self.lw_b, twd], [ps_w])
            self.MM(ps_a[:], self.la_b[:, cs_], twd[:], True, True, [self.la_b, twd], [ps_a])
            self.MM(ps_g[:], self.lg_b[:, cs_], sgd[:], True, True, [self.lg_b, sgd], [ps_g])
            lws, asg, cum, kk, nrm, kkn = N("lws"), N("asg"), N("cum"), N("kk"), N("nrm"), N("kkn")
            kk2 = N("kk2", BF16)
            kmod = N("kmod")
            tmp = self.shtmp[0]
            ea = Buf("ea_v", self.rawb[0][:, 0:T])
            ea.root = self.rawb[0]
            ecp = Buf("ecp_v", self.rawb[1][:, 0:T])
            ecp.root = self.rawb[1]
            tka, cl, bvec, ecm, ecl = nrm, tmp, kk, lws, ea
            kT = N("kT", BF16)
            rk2, vb = kk2, kT
            bh = Buf("bh_v", kS[:, 0:T // 2].bitcast(BF16))
            bh.root = kS
            kh = Buf("kh_v", kS[:, T // 2:T].bitcast(BF16))
            kh.root = kS
            col = lambda nm: pv[:, PV[nm] + hp:PV[nm] + hp + 1]
            self.act(lws, lws[:], ps_w, ps_w[:], AF.Sigmoid, bias=col("w0"), extra_reads=[pv])
            self.act(asg, asg[:], ps_a, ps_a[:], AF.Sigmoid, bias=col("a0"), extra_reads=[pv])
            self.act(g_sb, g_sb[:], ps_g, ps_g[:], AF.Copy)
            self.V(lambda e: e.tensor_tensor_scan(out=cum[:], data0=self.rmask[:], data1=lws[:], initial=0.0, op0=ALU.mult, op1=ALU.add),
                   [self.rmask, lws], [cum])
            self.A(lambda e: e.activation(out=kk[:], in_=kS[:], func=AF.Copy, scale=col("kk")), [kS, pv], [kk])
            self.act(kk2, kk2[:], kk, kk[:], AF.Square)
            ps_ss = self.next_ps()
            self.MM(ps_ss[:], self.bd_b[:], kk2[:], True, True, [self.bd_b, kk2], [ps_ss])
            self.act(nrm, nrm[:], ps_ss, ps_ss[:], AF.Sqrt)
            self.V(lambda e: e.tensor_scalar(out=nrm[:], in0=nrm[:], scalar1=1e-12, scalar2=None, op0=ALU.max), [nrm], [nrm])
            self.V(lambda e: e.reciprocal(out=nrm[:], in_=nrm[:]), [nrm], [nrm])
            self.V(lambda e: e.tensor_tensor(out=kkn[:], in0=kk[:], in1=nrm[:], op=ALU.mult), [kk, nrm], [kkn])
            self.V(lambda e: e.tensor_scalar(out=tka[:], in0=asg[:], scalar1=col("ka"), scalar2=self.onemka[:, hp:hp + 1], op0=ALU.mult, op1=ALU.add),
                   [asg, pv, self.onemka], [tka])
            self.V(lambda e: e.tensor_tensor(out=kmod[:], in0=kS[:], in1=tka[:], op=ALU.mult), [kS, tka], [kmod])
            self.G(lambda e: e.tensor_tensor(out=bvec[:], in0=kkn[:], in1=asg[:], op=ALU.mult), [kkn, asg], [bvec])
            self.V(lambda e: e.tensor_tensor(out=tmp[:], in0=cum[:], in1=lws[:], op=ALU.subtract), [cum, lws], [tmp])
            self.act(ea, ea[:], tmp, tmp[:], AF.Exp, scale=-CDEC)
            self.V(lambda e: e.scalar_tensor_tensor(out=arT[:, 0, :], in0=kkn[:], scalar=-1.0, in1=ea[:], op0=ALU.mult, op1=ALU.mult), [kkn, ea], [arT])
            self.act(ecp, ecp[:], cum, cum[:], AF.Exp, scale=-CDEC)
            self.V(lambda e: e.tensor_tensor(out=arT[:, 1, :], in0=rS[:], in1=ecp[:], op=ALU.mult), [rS, ecp], [arT])
            self.G(lambda e: e.tensor_copy(out=wl[:].unsqueeze(2), in_=ecp[:].rearrange("p (c l) -> p c l", l=64)[:, :, 63:64]), [ecp], [wl])
            self.act(ecm, ecm[:], cum, cum[:], AF.Exp, scale=CDEC)
            self.V(lambda e: e.tensor_tensor(out=bT[:], in0=bvec[:], in1=ecm[:], op=ALU.mult), [bvec, ecm], [bT])
            self.G(lambda e: e.tensor_tensor(out=kT[:], in0=kmod[:], in1=ecm[:], op=ALU.mult), [kmod, ecm], [kT])
            cv = cum[:].rearrange("p (c l) -> p c l", l=64)
            self.V(lambda e: e.tensor_tensor(out=cl[:].rearrange("p (c l) -> p c l", l=64), in0=cv[:, :, 63:64].to_broadcast([128, 8, 64]), in1=cv, op=ALU.subtract),
                   [cum], [cl])
            self.act(ecl, ecl[:], cl, cl[:], AF.Exp, scale=-CDEC)
            self.V(lambda e: e.tensor_tensor(out=bh[:], in0=bvec[:], in1=ecl[:], op=ALU.mult), [bvec, ecl], [bh])
            self.V(lambda e: e.tensor_tensor(out=kh[:], in0=kmod[:], in1=ecl[:], op=ALU.mult), [kmod, ecl], [kh])
            for e_ in range(2):
                hm = hmask[:, e_:e_ + 1]
                self.A(lambda e, e_=e_, hm=hm: e.activation(out=pad[("a", e_)][:], in_=arT[:, 0, :], func=AF.Copy, scale=hm), [arT, hmask], [pad[("a", e_)]])
                self.A(lambda e, e_=e_, hm=hm: e.activation(out=pad[("b", e_)][:], in_=bT[:], func=AF.Copy, scale=hm), [bT, hmask], [pad[("b", e_)]])
                self.A(lambda e, e_=e_, hm=hm: e.activation(out=pad[("k", e_)][:], in_=kT[:], func=AF.Copy, scale=hm), [kT, hmask], [pad[("k", e_)]])
            self.act(vb, vb[:], vS, vS[:], AF.Copy)
            self.G(lambda e: e.tensor_tensor(out=tmp[:], in0=rS[:], in1=kmod[:], op=ALU.mult), [rS, kmod], [tmp])
            self.A(lambda e: e.activation(out=rk2[:], in_=tmp[:], func=AF.Copy, scale=col("rk")), [tmp, pv], [rk2])
            ps_b = self.next_ps()
            self.MM(ps_b[:], self.bd_b[:], rk2[:], True, True, [self.bd_b, rk2], [ps_b])
            self.V(lambda e: e.tensor_tensor(out=bonus[:], in0=ps_b[:], in1=vS[:], op=ALU.mult), [ps_b, vS], [bonus])
            if getattr(self, "rw_stage", 9) < 1:
                return
            for qi, (src, dsts) in enumerate([(vb, [(tokV, None)]), (bh, [(tokB[0], 0), (tokB[1], 1)]), (kh, [(tokK[0], 0), (tokK[1], 1)]), (None, [(tokA, None)])]):
                bank = self.next_ps()
                tv = Buf("tv", bank[:, 0:256].bitcast(BF16))
                tv.root = bank
                for tb in range(4):
                    if src is None:
                        self.p.op("tensor", lambda e, tb=tb, tv=tv: e.transpose(out=tv[:, tb * 128:(tb + 1) * 128], in_=arT[:, 0, tb * 128:(tb + 1) * 128], identity=self.ident_b[:]),
                                  [arT, self.ident_b], [tv])
                    else:
                        self.p.op("tensor", lambda e, tb=tb, src=src, tv=tv: e.transpose(out=tv[:, tb * 128:(tb + 1) * 128], in_=src[:, tb * 128:(tb + 1) * 128], identity=self.ident_b[:]),
                                  [src, self.ident_b], [tv])
                for (dst, j) in dsts:
                    if j is None:
                        self.act(dst, dst[:].rearrange("p a b -> p (a b)"), tv, tv[:], AF.Copy)
                    else:
                        self.V(lambda e, dst=dst, j=j, tv=tv: e.tensor_scalar(out=dst[:].rearrange("p a b -> p (a b)"), in0=tv[:], scalar1=hmask[:, j:j + 1], scalar2=None, op0=ALU.mult),
                               [tv, hmask], [dst])
        if getattr(self, "rw_stage", 9) < 2:
            return
        with p.scope():
            PT = [N("PT%d" % u, BF16, (128, 128)) for u in range(8)]
            QQ = [[N("QQ%d_%d" % (u, i), BF16, (128, 256)) for i in range(2)] for u in range(8)]
            GG = [[N("GG%d_%d" % (u, i), BF16, (128, 128)) for i in range(2)] for u in range(8)]
            for rnd in range(2):
                for ui in range(4):
                    u = rnd * 4 + ui
                    tb, e_ = u // 2, u % 2
                    tbs = slice(tb * 128, (tb + 1) * 128)
                    bank = self.banks[ui]
                    self.MM(bank[:, 0:256], pad[("b", e_)][:, tbs], arT[:, :, tbs], True, True, [pad[("b", e_)], arT], [bank])
                    self.MM(bank[:, 256:512], pad[("k", e_)][:, tbs], arT[:, :, tbs], True, True, [pad[("k", e_)], arT], [bank])
                for ui in range(4):
                    u = rnd * 4 + ui
                    bank = self.banks[ui]
                    self.V(lambda e, u=u, bank=bank: e.tensor_tensor(out=A4[u][:], in0=bank[:], in1=self.mask4[:], op=ALU.mult), [bank, self.mask4], [A4[u]])
            for u in range(8):
                tb, e_ = u // 2, u % 2
                tbs = slice(tb * 128, (tb + 1) * 128)
                qv = self.q[4 + u // 4][u % 4]
                self.MM(qv[:], pad[("a", e_)][:, tbs], bT[:, tbs], True, True, [pad[("a", e_)], bT], [qv])
            for u in range(8):
                qv = self.q[4 + u // 4][u % 4]
                self.V(lambda e, u=u, qv=qv: e.tensor_tensor(out=PT[u][:], in0=qv[:], in1=self.m_sl[:], op=ALU.mult), [qv, self.m_sl], [PT[u]])
                self.G(lambda e, u=u: e.tensor_tensor(out=GG[u][0][:], in0=A4[u][:, 0:128], in1=self.ident_b[:], op=ALU.add), [A4[u], self.ident_b], [GG[u][0]])
            Qc = [(A4[u], A4[u][:, 0:128]) for u in range(8)]
            QTc = [(PT[u], PT[u][:]) for u in range(8)]
            for i in range(1, 6):
                for u in range(8):
                    hv = self.h[u // 2][u % 2]
                    if i < 5:
                        self.MM(hv[:, 0:128], QTc[u][1], Qc[u][1], True, True, [QTc[u][0], Qc[u][0]], [hv])
                    self.MM(hv[:, 128:256], Qc[u][1], QTc[u][1], True, True, [QTc[u][0], Qc[u][0]], [hv])
                for u in range(8):
                    hv = self.h[u // 2][u % 2]
                    qq = QQ[u][i % 2]
                    if i < 5:
                        self.act(qq, qq[:], hv, hv[:], AF.Copy)
                    else:
                        self.act(qq, qq[:, 128:256], hv, hv[:, 128:256], AF.Copy)
                    Qc[u] = (qq, qq[:, 0:128])
                    QTc[u] = (qq, qq[:, 128:256])
                for u in range(8):
                    qv = self.q[4 + u // 4][u % 4]
                    self.MM(qv[:], QTc[u][1], GG[u][(i - 1) % 2][:], True, True, [QTc[u][0], GG[u][(i - 1) % 2]], [qv])
                for u in range(8):
                    qv = self.q[4 + u // 4][u % 4]
                    dst = GG[u][i % 2] if i < 5 else TTm[u]
                    self.V(lambda e, u=u, qv=qv, dst=dst, i=i: e.tensor_tensor(out=dst[:], in0=qv[:], in1=GG[u][(i - 1) % 2][:], op=ALU.add), [qv, GG[u][(i - 1) % 2]], [dst])
            AVs = [N("AVs%d" % u, BF16, (128, 64)) for u in range(8)]
            for u in range(8):
                tb, e_ = u // 2, u % 2
                qv = self.q[u // 4][u % 4]
                self.MM(qv[:, 0:64], A4[u][:, 256:384], tokV[:, tb, e_ * 64:(e_ + 1) * 64], True, True, [A4[u], tokV], [qv])
            for u in range(8):
                qv = self.q[u // 4][u % 4]
                self.act(AVs[u], AVs[u][:], qv, qv[:, 0:64], AF.Copy)
            for u in range(8):
                tb, e_ = u // 2, u % 2
                qv = self.q[2 + u // 4][u % 4]
                self.MM(qv[:, 0:64], TTm[u][:], AVs[u][:], True, True, [TTm[u], AVs[u]], [qv])
                qa = self.q[4 + u // 4][u % 4]
                self.MM(qa[:], tokA[:, tb, :], TTm[u][:], True, True, [tokA, TTm[u]], [qa])
            for u in range(8):
                tb, e_ = u // 2, u % 2
                oc = slice(e_ * 64, (e_ + 1) * 64)
                qv = self.q[2 + u // 4][u % 4]
                self.V(lambda e, qv=qv, tb=tb, oc=oc: e.tensor_copy(out=U0[:, tb, oc], in_=qv[:, 0:64]), [qv], [U0])
                qa = self.q[4 + u // 4][u % 4]
                self.A(lambda e, qa=qa, tb=tb, oc=oc: e.activation(out=apT[oc, tb, :], in_=qa[oc, :], func=AF.Copy), [qa], [apT])
            if last and hp == 0:
                self.debug_out("rw_A4", A4[0], A4[0][:], [128, 512], BF16)
                self.debug_out("rw_TT", TTm[0], TTm[0][:], [128, 128], BF16)
                self.debug_out("rw_PT", PT[0], PT[0][:], [128, 128], BF16)
        if getattr(self, "rw_stage", 9) < 3:
            return
        step = 0
        for tb in range(4):
            tbs = slice(tb * 128, (tb + 1) * 128)
            for j in range(2):
                jb = j * 64
                cs = slice(tb * 128 + jb, tb * 128 + jb + 64)
                cidx = tb * 2 + j
                bi = step % 2
                step += 1
                q0, q1, q2, q3 = self.q[bi]
                us = [tb * 2, tb * 2 + 1]
                self.MM(q1[:], apT[:, tb, :], H2b[:], True, False, [apT, H2b], [q1])
                self.MM(q1[:], self.ident_b[:], U0[:, tb, :], False, True, [self.ident_b, U0], [q1])
                self.V(lambda e, q1=q1, jb=jb: e.tensor_copy(out=U_sb[jb:jb + 64, :], in_=q1[jb:jb + 64, :]), [q1], [U_sb])
                for e_ in range(2):
                    oc = slice(e_ * 64, (e_ + 1) * 64)
                    self.MM(q2[:, oc], H2b[:], arT[:, 1, cs], True, False, [H2b, arT], [q2])
                    self.MM(q2[:, oc], U_sb[:], A4[us[e_]][:, 128 + jb:128 + jb + 64], False, False, [U_sb, A4[us[e_]]], [q2])
                    self.MM(q2[:, oc], tokV[:, tb, :], A4[us[e_]][:, 384 + jb:384 + jb + 64], False, True, [tokV, A4[us[e_]]], [q2])
                self.MM(q3[:], tokB[j][:, tb, :], U_sb[:], True, False, [tokB[j], U_sb], [q3])
                self.MM(q3[:], tokK[j][:, tb, :], tokV[:, tb, :], False, True, [tokK[j], tokV], [q3])
                for e_ in range(2):
                    oc = slice(e_ * 64, (e_ + 1) * 64)
                    self.V(lambda e, oc=oc, q3=q3, cidx=cidx: e.scalar_tensor_tensor(out=H2f[oc, oc], in0=H2f[oc, oc], scalar=wl[oc, cidx:cidx + 1], in1=q3[oc, oc],
                                                                                   op0=ALU.mult, op1=ALU.add), [H2f, wl, q3], [H2f])
                    self.A(lambda e, oc=oc: e.activation(out=H2b[oc, oc], in_=H2f[oc, oc], func=AF.Copy), [H2f], [H2b])
                for e_ in range(2):
                    oc = slice(e_ * 64, (e_ + 1) * 64)
                    self.A(lambda e, oc=oc, q2=q2, cs=cs: e.activation(out=y_sb[oc, cs], in_=q2[oc, oc], func=AF.Copy), [q2], [y_sb])
        if last and hp == 0:
            self.debug_out("rw_y", y_sb, y_sb[:], [128, T])
        if getattr(self, "rw_stage", 9) < 4:
            return
        with p.scope():
            ysq, m_sb, yc, m2, var = (N(n) for n in ["ysq", "m_sb", "yc", "m2", "var"])
            ps_m, ps_q = self.next_ps(), self.next_ps()
            self.MM(ps_m[:], self.bd64[:], y_sb[:], True, True, [self.bd64, y_sb], [ps_m])
            self.act(ysq, ysq[:], y_sb, y_sb[:], AF.Square)
            self.MM(ps_q[:], self.bd64[:], ysq[:], True, True, [self.bd64, ysq], [ps_q])
            self.act(m_sb, m_sb[:], ps_m, ps_m[:], AF.Copy)
            self.G(lambda e: e.tensor_tensor(out=yc[:], in0=y_sb[:], in1=m_sb[:], op=ALU.subtract), [y_sb, m_sb], [yc])
            self.G(lambda e: e.tensor_tensor(out=m2[:], in0=m_sb[:], in1=m_sb[:], op=ALU.mult), [m_sb], [m2])
            self.V(lambda e: e.tensor_tensor(out=var[:], in0=ps_q[:], in1=m2[:], op=ALU.subtract), [ps_q, m2], [var])
            self.V(lambda e: e.tensor_scalar(out=var[:], in0=var[:], scalar1=0.0, scalar2=None, op0=ALU.max), [var], [var])
            self.act(var, var[:], var, var[:], AF.Sqrt, bias=self.eps_col[:, 1:2], extra_reads=[self.eps_col])
            self.V(lambda e: e.reciprocal(out=var[:], in_=var[:]), [var], [var])
            self.G(lambda e: e.tensor_tensor(out=yc[:], in0=yc[:], in1=var[:], op=ALU.mult), [yc, var], [yc])
            self.V(lambda e: e.tensor_scalar(out=yc[:], in0=yc[:], scalar1=pv[:, PV["lng"] + hp:PV["lng"] + hp + 1], scalar2=pv[:, PV["lnb"] + hp:PV["lnb"] + hp + 1],
                                             op0=ALU.mult, op1=ALU.add), [yc, pv], [yc])
            self.G(lambda e: e.tensor_tensor(out=yc[:], in0=yc[:], in1=bonus[:], op=ALU.add), [yc, bonus], [yc])
            self.V(lambda e: e.tensor_tensor(out=rwo[:, hp, :], in0=yc[:], in1=g_sb[:], op=ALU.mult), [yc, g_sb], [rwo])


def prep_shared(inp):
    f = lambda a: np.ascontiguousarray(np.asarray(a, dtype=np.float32))
    L = 0
    pv = np.zeros((128, NPV), np.float32)

    def put(name, vec, ntile):
        v = np.asarray(vec, np.float32).reshape(ntile, 128)
        pv[:, PV[name]:PV[name] + ntile] = v.T
    put("mix", inp["mix_norm"][L], 8)
    put("ffn", inp["ffn_norm"][L], 8)
    put("ple", inp["ple_norm"][L], 8)
    put("fin", inp["final_norm"], 8)
    put("mu", inp["mu_shift"][L], 14)
    put("w0", inp["rk_w0"][L], 4)
    put("a0", inp["rk_a0"][L], 4)
    put("kk", inp["rk_k_k"][L], 4)
    put("ka", inp["rk_k_a"][L], 4)
    put("rk", np.asarray(inp["rk_r_k"][L]).reshape(512), 4)
    put("lng", inp["rk_ln_g"][L], 4)
    put("lnb", inp["rk_ln_b"][L], 4)
    put("s5d", np.asarray(inp["s5_d"][L]).reshape(256), 2)
    put("glub", inp["s5_glu_b"][L], 2)
    lre = np.asarray(inp["s5_lam_re"][L], np.float32)
    lim = np.asarray(inp["s5_lam_im"][L], np.float32)
    ldt = np.asarray(inp["s5_log_dt"][L], np.float32)
    for gp in range(8):
        for g2 in range(2):
            g = 2 * gp + g2
            pv[g2 * 64:(g2 + 1) * 64, PV["lre"] + gp] = lre[g]
            pv[g2 * 64:(g2 + 1) * 64, PV["lim"] + gp] = lim[g]
            pv[g2 * 64:(g2 + 1) * 64, PV["ldt"] + gp] = ldt[g]
    bre = np.asarray(inp["s5_b_re"][L], np.float32)
    bim = np.asarray(inp["s5_b_im"][L], np.float32)
    cre = np.asarray(inp["s5_c_re"][L], np.float32)
    cim = np.asarray(inp["s5_c_im"][L], np.float32)
    Bre = np.zeros((256, 512), np.float32)
    Bim = np.zeros((256, 512), np.float32)
    Cre = np.zeros((128, 8, 128), np.float32)
    Cim = np.zeros((128, 8, 128), np.float32)
    for g in range(16):
        col0 = ((g % 8) // 2) * 128 + (g % 2) * 64
        Bre[g * 16:(g + 1) * 16, col0:col0 + 64] = bre[g].T
        Bim[g * 16:(g + 1) * 16, col0:col0 + 64] = bim[g].T
        gp, g2 = g // 2, g % 2
        Cre[g2 * 64:(g2 + 1) * 64, gp, (g % 8) * 16:(g % 8) * 16 + 16] = cre[g].T
        Cim[g2 * 64:(g2 + 1) * 64, gp, (g % 8) * 16:(g % 8) * 16 + 16] = cim[g].T
    lora_wa = np.concatenate([np.asarray(inp["rk_w_up"][L]), np.asarray(inp["rk_a_up"][L])], axis=0)
    wr = np.concatenate([np.asarray(inp["router_group_w"][L]), np.asarray(inp["router_expert_w"][L])], axis=1)
    rb = np.concatenate([np.asarray(inp["router_group_b"][L]), np.asarray(inp["router_expert_b"][L])], axis=0)
    return {
        "pvec": pv, "w_in": f(inp["w_in"][L]), "lora_wa": f(lora_wa), "lora_g": f(inp["rk_g_up"][L]),
        "glu_w": f(inp["s5_glu_w"][L]), "w_ba": f(inp["w_branch_a"][L]), "w_bb": f(inp["w_branch_b"][L]),
        "w_out": f(inp["w_out"][L]), "wr": f(wr), "rbias": f(np.broadcast_to(rb[None, :], (128, 36))),
        "wg": f(inp["exp_w_gate"][L]), "wu": f(inp["exp_w_up"][L]), "wd": f(inp["exp_w_down"][L]),
        "plg": f(inp["ple_gate_w"][L]), "plp": f(inp["ple_proj"][L]),
        "s5bre": Bre, "s5bim": Bim, "s5cre": f(Cre.reshape(128, 1024)), "s5cim": f(Cim.reshape(128, 1024)),
    }


def prep_core(inp, b0, nseq):
    x = np.asarray(inp["x"], np.float32)[b0:b0 + nseq]
    pp = np.asarray(inp["p"], np.float32)[0, b0:b0 + nseq]
    return {"xT": np.ascontiguousarray(x.transpose(0, 2, 1)), "pT": np.ascontiguousarray(pp.transpose(0, 2, 1))}


_CACHE = {}


def kernel(**inputs):
    B, S, _ = inputs["x"].shape
    nseq = B // NCORES
    key = (nseq, S)
    if key not in _CACHE:
        _CACHE[key] = FullBuilder(nseq, S).build()
    nc = _CACHE[key]
    shared = prep_shared(inputs)
    in_maps = []
    for cidx in range(NCORES):
        m = dict(shared)
        m.update(prep_core(inputs, cidx * nseq, nseq))
        in_maps.append(m)
    res = run_bass_kernel_spmd(nc, in_maps, core_ids=list(range(NCORES)))
    out = np.empty((B, S, D), np.float32)
    for cidx in range(NCORES):
        out[cidx * nseq:(cidx + 1) * nseq] = res.results[cidx]["outT"].transpose(0, 2, 1)
    return out
```
